# Optimizing a Trainium2 kernel written in Bass

```python
import jax, jax.numpy as jnp
from jax import lax
import numpy as np

D_MODEL = 1024
BATCH = 16
SEQ = 4096
DEPTH = 1

CHUNK = 64
EPS = 1e-6
N_HEADS = 8
HEAD_DIM = 64
ATTN_WIDTH = N_HEADS * HEAD_DIM
ROT_DIM = HEAD_DIM // 4
ROPE_THETA = 500000.0
IDX_HEADS = 8
IDX_DIM = 64
TOPK_MAX = 256
POOL_WINDOWS = (2, 4, 8, 16)
POOL_GROUPS = len(POOL_WINDOWS)
POOL_WIDTH = 512
POOL_GROUP_DIM = POOL_WIDTH // POOL_GROUPS
N_BRANCHES = 2
PEER_HEADS = 8
PEER_KEYS = 128
PEER_EXPERTS = PEER_KEYS * PEER_KEYS
PEER_KEY_DIM = 64
PEER_TOPK = 16
TOKEN_BLOCK = 128

SPLIT_SIZES = (ATTN_WIDTH, ATTN_WIDTH, ATTN_WIDTH, IDX_HEADS * IDX_DIM, IDX_DIM, IDX_HEADS, POOL_WIDTH, N_BRANCHES * D_MODEL)
SPLIT_POINTS = tuple(int(v) for v in np.cumsum(SPLIT_SIZES)[:-1])
IN_WIDTH = int(sum(SPLIT_SIZES))

kernel_name = "hybrid_dsa_pool_peer_block"


def rms_norm(x, g):
    xf = x.astype(jnp.float32)
    y = xf * lax.rsqrt(jnp.mean(xf * xf, axis=-1, keepdims=True) + EPS)
    return (y * g.astype(jnp.float32)).astype(x.dtype)


def partial_rope(x, pos):
    half = ROT_DIM // 2
    inv_freq = ROPE_THETA ** (-jnp.arange(half, dtype=jnp.float32) / half)
    ang = pos.astype(jnp.float32)[:, None] * inv_freq[None, :]
    cos = jnp.cos(ang)[None, :, None, :]
    sin = jnp.sin(ang)[None, :, None, :]
    xr = x[..., :ROT_DIM].astype(jnp.float32)
    x1, x2 = xr[..., :half], xr[..., half:]
    rot = jnp.concatenate([x1 * cos - x2 * sin, x2 * cos + x1 * sin], axis=-1)
    return jnp.concatenate([rot.astype(x.dtype), x[..., ROT_DIM:]], axis=-1)


def dsa_attention(q, k, v, q_i, k_i, w_i):
    B, S = q.shape[0], q.shape[1]
    n_blocks = S // CHUNK
    topk = min(TOPK_MAX, S // 4)
    key_pos = jnp.arange(S)

    def to_blocks(a):
        return jnp.moveaxis(a.reshape((B, n_blocks, CHUNK) + a.shape[2:]), 1, 0)

    def one_block(args):
        c, qb, qib, wib = args
        limit = (c + 1) * CHUNK
        admissible = key_pos < limit
        logits = jnp.einsum('bthd,bsd->bths', qib, k_i, preferred_element_type=jnp.float32) * (IDX_DIM ** -0.5)
        score = jnp.einsum('bth,bths->bts', wib.astype(jnp.float32), jax.nn.relu(logits))
        score = jnp.where(admissible[None, None, :], score, -jnp.inf)
        _, idx = lax.top_k(score, topk)
        valid = idx < limit
        k_sel = jax.vmap(lambda kb, ib: kb[ib])(k, idx)
        v_sel = jax.vmap(lambda vb, ib: vb[ib])(v, idx)
        s = jnp.einsum('bthd,btkhd->bthk', qb, k_sel, preferred_element_type=jnp.float32) * (HEAD_DIM ** -0.5)
        s = jnp.where(valid[:, :, None, :], s, -jnp.inf)
        p = jax.nn.softmax(s, axis=-1)
        return jnp.einsum('bthk,btkhd->bthd', p.astype(v.dtype), v_sel)

    out = lax.map(one_block, (jnp.arange(n_blocks), to_blocks(q), to_blocks(q_i), to_blocks(w_i)))
    return jnp.moveaxis(out, 0, 1).reshape(B, S, ATTN_WIDTH)


def multiscale_pool(p, pool_w, pool_scale):
    B, S, _ = p.shape
    pf = p.astype(jnp.float32).reshape(B, S, POOL_GROUPS, POOL_GROUP_DIM)
    csum = jnp.cumsum(pf, axis=1)
    t1 = jnp.arange(1, S + 1, dtype=jnp.float32)
    groups = []
    for g, w in enumerate(POOL_WINDOWS):
        c = csum[:, :, g]
        lower = jnp.concatenate([jnp.zeros((B, w, POOL_GROUP_DIM), jnp.float32), c[:, :S - w]], axis=1)
        count = jnp.minimum(t1, float(w))[None, :, None]
        groups.append((c - lower) / count - pf[:, :, g])
    pooled = jnp.stack(groups, axis=2).astype(p.dtype)
    mixed = jnp.einsum('bsgc,gcd->bsgd', pooled, pool_w)
    return mixed.reshape(B, S, POOL_WIDTH) * pool_scale


def peer_ffn(h, wq, subkeys, u, v):
    B, S, D = h.shape
    hb = h.reshape((B * S) // TOKEN_BLOCK, TOKEN_BLOCK, D)

    def one_block(xb):
        tb = xb.shape[0]
        q = (xb @ wq).reshape(tb, PEER_HEADS, 2, PEER_KEY_DIM)
        sub = jnp.einsum('thpd,hpnd->thpn', q, subkeys, preferred_element_type=jnp.float32)
        s_half, i_half = lax.top_k(sub, PEER_TOPK)
        cand = (s_half[:, :, 0, :, None] + s_half[:, :, 1, None, :]).reshape(tb, PEER_HEADS, PEER_TOPK * PEER_TOPK)
        cand_idx = (i_half[:, :, 0, :, None] * PEER_KEYS + i_half[:, :, 1, None, :]).reshape(tb, PEER_HEADS, PEER_TOPK * PEER_TOPK)
        top_s, pos = lax.top_k(cand, PEER_TOPK)
        expert = jnp.take_along_axis(cand_idx, pos, axis=-1)
        g = jax.nn.softmax(top_s, axis=-1)
        u_sel = u[expert]
        v_sel = v[expert]
        a = jax.nn.gelu(jnp.einsum('td,thkd->thk', xb, u_sel, preferred_element_type=jnp.float32), approximate=False)
        return jnp.einsum('thk,thkd->td', (g * a).astype(v.dtype), v_sel)

    return lax.map(one_block, hb).reshape(B, S, D)


def setup_inputs(seed: int = 0) -> dict:
    key = jax.random.key(seed)
    ks = jax.random.split(key, 16)
    f32 = jnp.float32
    L = DEPTH
    nrm = lambda k, shape, scale: jax.random.normal(k, shape, f32) * scale
    return {
        "x": jax.random.normal(ks[0], (BATCH, SEQ, D_MODEL), f32),
        "norm1_g": 1.0 + nrm(ks[1], (L, D_MODEL), 0.02),
        "w_in": nrm(ks[2], (L, D_MODEL, IN_WIDTH), D_MODEL ** -0.5),
        "q_norm_g": 1.0 + nrm(ks[3], (L, HEAD_DIM), 0.02),
        "k_norm_g": 1.0 + nrm(ks[4], (L, HEAD_DIM), 0.02),
        "pool_w": nrm(ks[5], (L, POOL_GROUPS, POOL_GROUP_DIM, POOL_GROUP_DIM), POOL_GROUP_DIM ** -0.5),
        "pool_scale": 1.0 + nrm(ks[6], (L, POOL_WIDTH), 0.02),
        "w_branch_attn": nrm(ks[7], (L, ATTN_WIDTH, D_MODEL), ATTN_WIDTH ** -0.5),
        "w_branch_pool": nrm(ks[8], (L, POOL_WIDTH, D_MODEL), POOL_WIDTH ** -0.5),
        "w_out": nrm(ks[9], (L, D_MODEL, D_MODEL), D_MODEL ** -0.5),
        "norm2_g": 1.0 + nrm(ks[10], (L, D_MODEL), 0.02),
        "peer_wq": nrm(ks[11], (L, D_MODEL, PEER_HEADS * 2 * PEER_KEY_DIM), D_MODEL ** -0.5),
        "peer_subkeys": nrm(ks[12], (L, PEER_HEADS, 2, PEER_KEYS, PEER_KEY_DIM), PEER_KEY_DIM ** -0.5),
        "peer_u": nrm(ks[13], (L, PEER_EXPERTS, D_MODEL), D_MODEL ** -0.5),
        "peer_v": nrm(ks[14], (L, PEER_EXPERTS, D_MODEL), PEER_HEADS ** -0.5),
    }


def reference(x, norm1_g, w_in, q_norm_g, k_norm_g, pool_w, pool_scale, w_branch_attn, w_branch_pool, w_out, norm2_g, peer_wq, peer_subkeys, peer_u, peer_v):
    B, S, _ = x.shape
    pos = jnp.arange(S)
    for l in range(DEPTH):
        xn = rms_norm(x, norm1_g[l])
        proj = xn @ w_in[l]
        q, k, v, qi, ki, wi, p, gl = jnp.split(proj, SPLIT_POINTS, axis=-1)
        q = partial_rope(rms_norm(q.reshape(B, S, N_HEADS, HEAD_DIM), q_norm_g[l]), pos)
        k = partial_rope(rms_norm(k.reshape(B, S, N_HEADS, HEAD_DIM), k_norm_g[l]), pos)
        v = v.reshape(B, S, N_HEADS, HEAD_DIM)
        qi = partial_rope(qi.reshape(B, S, IDX_HEADS, IDX_DIM), pos)
        ki = partial_rope(ki.reshape(B, S, 1, IDX_DIM), pos)[:, :, 0]
        wi = wi * (IDX_HEADS ** -0.5)
        y_attn = dsa_attention(q, k, v, qi, ki, wi) @ w_branch_attn[l]
        y_pool = multiscale_pool(p, pool_w[l], pool_scale[l]) @ w_branch_pool[l]
        g_attn, g_pool = jnp.split(jax.nn.sigmoid(gl), 2, axis=-1)
        x = x + (g_attn * y_attn + g_pool * y_pool) @ w_out[l]
        x = x + peer_ffn(rms_norm(x, norm2_g[l]), peer_wq[l], peer_subkeys[l], peer_u[l], peer_v[l])
    return x
```

```python
import bisect
from contextlib import ExitStack

import numpy as np
import ml_dtypes
import concourse.bass as bass
import concourse.mybir as mybir
from concourse.bass_utils import run_bass_kernel_spmd

F32 = mybir.dt.float32
BF16 = mybir.dt.bfloat16
U32 = mybir.dt.uint32
AF = mybir.ActivationFunctionType
ALU = mybir.AluOpType
AX = mybir.AxisListType

D = 1024
EPS = 1e-6
NEG = -1.0e30


class Sched:
    def __init__(self, nc, es):
        self.nc = nc
        self.es = es
        self.E = dict(pe=nc.tensor, dve=nc.vector, act=nc.scalar, pool=nc.gpsimd, sp=nc.sync)
        self.sem = {k: es.enter_context(nc.semaphore("s_" + k)) for k in self.E}
        self.ins = {k: [] for k in self.E}
        self.marks = {k: [] for k in self.E}
        self.waited = {k: {} for k in self.E}
        self.res = {}
        self.dsem = {}

    def _resolve(self, dep):
        if dep[0] == 'd':
            return ('d', dep[1]), self.dsem[dep[1]][0], dep[2]
        _, e, idx = dep
        m = self.marks[e]
        if m and m[-1] >= idx:
            pos = bisect.bisect_left(m, idx)
            return ('c', e), self.sem[e], pos + 1
        self.ins[e][idx].then_inc(self.sem[e], 1)
        m.append(idx)
        return ('c', e), self.sem[e], len(m)

    def _wait(self, eng, deps):
        for dep in deps:
            if dep is None:
                continue
            if dep[0] == 'c' and dep[1] == 'pe' and eng == 'pe':
                continue
            key, sem, val = self._resolve(dep)
            if self.waited[eng].get(key, 0) >= val:
                continue
            self.E[eng].wait_ge(sem, val)
            self.waited[eng][key] = val

    def _deps(self, reads, writes):
        deps = []
        for r in reads:
            st = self.res.get(r)
            if st and st['w'] is not None:
                deps.append(st['w'])
        for w in writes:
            st = self.res.get(w)
            if st:
                if st['w'] is not None:
                    deps.append(st['w'])
                deps.extend(st['r'].values())
        return deps

    def _update(self, dep, rkey, reads, writes):
        for r in reads:
            st = self.res.setdefault(r, {'w': None, 'r': {}})
            st['r'][rkey] = dep
        for w in writes:
            self.res[w] = {'w': dep, 'r': {}}

    def op(self, eng, fn, reads=(), writes=()):
        self._wait(eng, self._deps(reads, writes))
        ins = fn()
        idx = len(self.ins[eng])
        self.ins[eng].append(ins)
        self._update(('c', eng, idx), eng, reads, writes)
        return ins

    def dma(self, q, out, in_, reads=(), writes=(), slot=None):
        deps = self._deps(reads, writes)
        if slot not in self.dsem:
            self.dsem[slot] = [self.es.enter_context(self.nc.semaphore("d_" + slot)), 0]
        s = self.dsem[slot]
        if s[1] > 0:
            deps.append(('d', slot, 16 * s[1]))
        self._wait(q, deps)
        s[1] += 1
        self.E[q].dma_start(out=out, in_=in_).then_inc(s[0], 16)
        self._update(('d', slot, 16 * s[1]), ('d', slot), reads, writes)

    def barrier(self):
        deps = []
        for e in self.E:
            if self.ins[e]:
                deps.append(('c', e, len(self.ins[e]) - 1))
        for slot, s in self.dsem.items():
            if s[1] > 0:
                deps.append(('d', slot, 16 * s[1]))
        for e in self.E:
            for dep in deps:
                key, sem, val = self._resolve(dep)
                if self.waited[e].get(key, 0) >= val:
                    continue
                self.E[e].wait_ge(sem, val)
                self.waited[e][key] = val
        self.res = {}


def AP(t, off, dims):
    return bass.AP(t, off, [list(d) for d in dims])


def build(NB, S, phases=("mix", "peer"), debug=False, stage=99):
    NT = NB * S
    NTILE = NT // 128
    TPS = S // 128
    nc = bass.Bass("TRN2", target_bir_lowering=False)

    def din(name, shape, dt=F32):
        return nc.dram_tensor(name, list(shape), dt, kind="ExternalInput")

    x_d = din("x", [NT, D])
    y_d = nc.dram_tensor("y", [NT, D], F32, kind="ExternalOutput")
    g2_d = din("g2", [128, 8])
    wq_d = din("wq", [D, D])
    skt_d = din("skt", [128, 16, 128])
    u_d = din("pu", [16384, D])
    v_d = din("pv", [16384, D])
    identb_d = din("identb", [128, 128], BF16)
    iota128_d = din("iota128", [128, 128], BF16)
    c16_d = din("c16", [128, 3, 16])
    g1_d = din("g1", [128, 8])
    win_d = din("win", [D, 4680])
    gqk_d = din("gqk", [128, 2, 64])
    rope_d = din("rope", [S // 128, 128, 2, 8])
    poolw_d = din("poolw", [128, 4, 128])
    pscale_d = din("pscale", [128, 4])
    wba_d = din("wba", [512, D])
    wbp_d = din("wbp", [512, D])
    wout_d = din("wout", [D, D])
    band_d = din("band", [3, 128, 4, 128])
    bmask_d = din("bmask", [128, 128])

    def dscr(name, shape, dt):
        return nc.dram_tensor(name, list(shape), dt, kind="Internal")

    ut_s = dscr("ut_s", [128, 128, D], BF16)
    v_s = dscr("v_s", [128, 128, D], BF16)
    h_s = dscr("h_s", [NT, D], F32)
    qT_s = dscr("qT_s", [NTILE, 128, 1024], BF16)
    kT_s = dscr("kT_s", [NB, 128, 4, S], BF16)
    v2_s = dscr("v2_s", [NB, S, 520], BF16)
    qiT_s = dscr("qiT_s", [NTILE, 128, 1024], BF16)
    kiT_s = dscr("kiT_s", [NB, 128, S], BF16)
    wi_s = dscr("wi_s", [NTILE, 128, 8], F32)
    ga_s = dscr("ga_s", [NTILE, 128, D], BF16)
    zp_s = dscr("zp_s", [NTILE, 128, D], BF16)
    dbg = {}

    es = ExitStack()
    with es:
        es.enter_context(nc.allow_low_precision(reason="bf16 matmul operands by design"))
        es.enter_context(nc.allow_non_contiguous_dma(reason="layout transforms"))
        S_ = Sched(nc, es)
        global LAST_SCHED
        LAST_SCHED = S_
        op = S_.op
        dma = S_.dma
        V, A_, P_, T_ = nc.vector, nc.scalar, nc.gpsimd, nc.tensor

        def sb(st, name, shape, dt):
            return st.enter_context(nc.sbuf_tensor("sb_" + name, list(shape), dt))

        def ps(st, name, shape, dt):
            return st.enter_context(nc.psum_tensor("ps_" + name, list(shape), dt))

        cst = ExitStack()
        es.enter_context(cst)
        identb = sb(cst, "identb", [128, 128], BF16)
        iota128 = sb(cst, "iota128", [128, 128], BF16)
        c16 = sb(cst, "c16", [128, 3, 16], F32)
        g2 = sb(cst, "g2", [128, 8], F32)
        g1 = sb(cst, "g1", [128, 8], F32)
        dma('sp', identb[:], identb_d.ap(), writes=["identb"], slot="c0")
        dma('sp', iota128[:], iota128_d.ap(), writes=["iota128"], slot="c1")
        dma('sp', c16[:], c16_d.ap(), writes=["c16"], slot="c2")
        dma('sp', g2[:], g2_d.ap(), writes=["g2"], slot="c3")
        dma('sp', g1[:], g1_d.ap(), writes=["g1"], slot="c4")

        banks = [ps(cst, f"bank{i}", [128, 512], F32) for i in range(8)]
        BK = [f"B{i}" for i in range(8)]

        def bank_bf(i):
            return banks[i][:].bitcast(BF16)

        def norm_transpose(st_tag, src, src_key, junk, ssq, rstd, xnb, g_t, g_key,
                           xnT_ap_fn, xnT_key, bank_i, op_=None):
            op = op_ or S_.op
            op('act', lambda: A_.activation(out=junk[:], in_=src[:], func=AF.Square,
                                            accum_out=ssq[:]),
               reads=[src_key], writes=[st_tag + "junk", st_tag + "ssq"])
            op('dve', lambda: V.tensor_scalar(out=rstd[:], in0=ssq[:], scalar1=1.0 / D,
                                              scalar2=EPS, op0=ALU.mult, op1=ALU.add),
               reads=[st_tag + "ssq"], writes=[st_tag + "rstd"])
            op('act', lambda: A_.activation(out=rstd[:], in_=rstd[:], func=AF.Sqrt),
               reads=[st_tag + "rstd"], writes=[st_tag + "rstd"])
            op('dve', lambda: V.reciprocal(out=rstd[:], in_=rstd[:]),
               reads=[st_tag + "rstd"], writes=[st_tag + "rstd"])
            op('dve', lambda: V.tensor_scalar(out=xnb[:], in0=src[:], scalar1=rstd[:, 0:1],
                                              scalar2=None, op0=ALU.mult),
               reads=[src_key, st_tag + "rstd"], writes=[st_tag + "xnb"])
            pb = bank_bf(bank_i)
            for kc in range(8):
                op('pe', lambda kc=kc: T_.transpose(out=pb[:, kc * 128:(kc + 1) * 128],
                                                    in_=xnb[:, kc * 128:(kc + 1) * 128],
                                                    identity=identb[:]),
                   reads=[st_tag + "xnb", "identb"], writes=[BK[bank_i]])
            pbv = AP(pb.tensor, pb.offset, [pb.ap[0], [128, 8], [1, 128]])
            gv = AP(g_t, 0, [[8, 128], [1, 8], [0, 128]])
            op('dve', lambda: V.tensor_tensor(out=xnT_ap_fn(), in0=pbv, in1=gv, op=ALU.mult),
               reads=[BK[bank_i], g_key], writes=[xnT_key])


        prep_done = [False]

        class Deferred:
            def __init__(self):
                self.q = []

            def op(self, *a, **k):
                self.q.append((op, a, k))

            def dma(self, *a, **k):
                self.q.append((dma, a, k))

            def run(self, n):
                while n > 0 and self.q:
                    f, a, k = self.q.pop(0)
                    f(*a, **k)
                    n -= 1

        def prep_ops(st, op, dma):
            prep_done[0] = True
            ub = [sb(st, f"ub{i}", [128, D], F32) for i in range(2)]
            ubb = [sb(st, f"ubb{i}", [128, D], BF16) for i in range(2)]
            utb = [sb(st, f"utb{i}", [128, D], BF16) for i in range(2)]
            vb = [sb(st, f"vb{i}", [128, D], F32) for i in range(2)]
            vbb = [sb(st, f"vbb{i}", [128, D], BF16) for i in range(2)]
            for c in range(128):
                b = c % 2
                dma('sp', ub[b][:], u_d.ap()[c * 128:(c + 1) * 128, :],
                    writes=[f"ub{b}"], slot=f"ub{b}")
                dma('sp', vb[b][:], v_d.ap()[c * 128:(c + 1) * 128, :],
                    writes=[f"vb{b}"], slot=f"vb{b}")
                op('dve', lambda b=b: V.tensor_copy(out=ubb[b][:], in_=ub[b][:]),
                   reads=[f"ub{b}"], writes=[f"ubb{b}"])
                op('pool', lambda b=b: P_.tensor_copy(out=vbb[b][:], in_=vb[b][:]),
                   reads=[f"vb{b}"], writes=[f"vbb{b}"])
                bi = 6 + b
                pb = bank_bf(bi)
                for kc in range(8):
                    op('pe', lambda kc=kc, b=b, pb=pb: T_.transpose(
                        out=pb[:, kc * 128:(kc + 1) * 128],
                        in_=ubb[b][:, kc * 128:(kc + 1) * 128], identity=identb[:]),
                       reads=[f"ubb{b}", "identb"], writes=[BK[bi]])
                op('act', lambda b=b, pb=pb: A_.copy(out=utb[b][:], in_=pb),
                   reads=[BK[bi]], writes=[f"utb{b}"])
                dma('pool', ut_s.ap()[c], utb[b][:], reads=[f"utb{b}"], writes=["ut_s"],
                    slot=f"uts{b}")
                dma('pool', v_s.ap()[c], vbb[b][:], reads=[f"vbb{b}"], writes=["v_s"],
                    slot=f"vs{b}")

        def peer_phase():
            T = 256
            NBLK = NT // T
            if not prep_done[0]:
                with ExitStack() as st:
                    prep_ops(st, op, dma)
                    S_.barrier()
            if stage <= 1:
                return

            with ExitStack() as st:
                wqb = sb(st, "wqb", [128, 8, D], BF16)
                sktb = sb(st, "sktb", [128, 16, 128], BF16)
                with ExitStack() as st2:
                    wtmp = sb(st2, "wtmp", [128, 8, D], F32)
                    stmp = sb(st2, "stmp", [128, 16, 128], F32)
                    dma('sp', wtmp[:], wq_d.ap().rearrange("(k p) c -> p k c", p=128),
                        writes=["wtmp"], slot="c5")
                    dma('sp', stmp[:], skt_d.ap(), writes=["stmp"], slot="c6")
                    op('dve', lambda: V.tensor_copy(out=wqb[:], in_=wtmp[:]),
                       reads=["wtmp"], writes=["wqb"])
                    op('dve', lambda: V.tensor_copy(out=sktb[:], in_=stmp[:]),
                       reads=["stmp"], writes=["sktb"])
                    S_.barrier()

                WT2 = sb(st, "WT2", [128, T, 128], BF16)
                NSB = 5
                utc = [sb(st, f"utc{i}", [128, D], BF16) for i in range(NSB)]
                vc = [sb(st, f"vc{i}", [128, D], BF16) for i in range(NSB)]
                xnTs = [sb(st, f"xnT{i}", [128, 8, T], BF16) for i in range(2)]
                hbs = [[sb(st, f"hb{i}{j}", [128, D], F32) for j in range(2)] for i in range(2)]
                junk = sb(st, "pjunk", [128, D], BF16)
                xnb = sb(st, "pxnb", [128, D], BF16)
                ssq = sb(st, "pssq", [128, 1], F32)
                rstd = sb(st, "prstd", [128, 1], F32)
                qT = sb(st, "pqT", [128, 8, T], BF16)
                sub = sb(st, "psub", [128, 16, 128], F32)
                sub2 = sb(st, "psub2", [128, 16, 128], F32)
                a16 = sb(st, "pa16", [128, 16, 16], F32)
                ixu = sb(st, "pixu", [128, 16, 16], U32)
                ixf = sb(st, "pixf", [128, 16, 16], F32)
                cand = sb(st, "pcand", [128, 8, 256], F32)
                cand2 = sb(st, "pcand2", [128, 8, 256], F32)
                ts = sb(st, "pts", [128, 8, 16], F32)
                posu = sb(st, "pposu", [128, 8, 16], U32)
                posf = sb(st, "pposf", [128, 128], F32)
                e0 = sb(st, "pe0", [128, 128, 16], F32)
                e1 = sb(st, "pe1", [128, 128, 16], F32)
                r0f = sb(st, "pr0f", [128, 128], F32)
                r1f = sb(st, "pr1f", [128, 128], F32)
                zs = sb(st, "pzs", [128, 8], F32)
                trin = sb(st, "ptrin", [128, 3, 128], BF16)
                trTs = [[sb(st, f"ptrT{i}{j}", [128, 3, 128], BF16) for j in range(2)] for i in range(2)]
                Lc = [sb(st, f"pLc{i}", [128, 8, 128], BF16) for i in range(2)]
                Rc = [sb(st, f"pRc{i}", [128, 8, 128], BF16) for i in range(2)]
                gact = [sb(st, f"pga{i}", [128, T], BF16) for i in range(3)]
                gw = [sb(st, f"pgw{i}", [128, T], BF16) for i in range(3)]

                def r1(blk, op, dma):
                    par = blk % 2
                    xnT = xnTs[par]
                    hb = hbs[par]
                    for tt in range(2):
                        tile = blk * 2 + tt
                        tk = f"hb{par}{tt}"
                        dma('sp', hb[tt][:], h_s.ap()[tile * 128:(tile + 1) * 128, :],
                            reads=["h_s"], writes=[tk], slot=tk)
                        norm_transpose("p", hb[tt], tk, junk, ssq, rstd, xnb, g2, "g2",
                                       lambda tt=tt: xnT[:, :, tt * 128:(tt + 1) * 128],
                                       f"xnT{par}", 7, op_=op)
                    for c in range(8):
                        bi = 7
                        for kc in range(8):
                            op('pe', lambda c=c, kc=kc, bi=bi: T_.matmul(
                                banks[bi][:, 0:T], lhsT=wqb[:, kc, c * 128:(c + 1) * 128],
                                rhs=xnT[:, kc, :], start=(kc == 0), stop=(kc == 7)),
                               reads=["wqb", f"xnT{par}"], writes=[BK[bi]])
                        op('act', lambda c=c, bi=bi: A_.copy(out=qT[:, c, :], in_=banks[bi][:, 0:T]),
                           reads=[BK[bi]], writes=["pqT"])
                    def tile_body(tt):
                        trT = trTs[par][tt]
                        for r in range(4):
                            bi = 7
                            for gg in range(4):
                                g = r * 4 + gg
                                h = g // 2
                                op('pe', lambda h=h, g=g, gg=gg, bi=bi, tt=tt: T_.matmul(
                                    banks[bi][:, gg * 128:(gg + 1) * 128],
                                    lhsT=qT[:, h, tt * 128:(tt + 1) * 128],
                                    rhs=sktb[:, g, :],
                                    start=True, stop=True),
                                   reads=["pqT", "sktb"], writes=[BK[bi]])
                            op('act', lambda bi=bi, r=r: A_.copy(
                                out=sub[:, r * 4:(r + 1) * 4, :], in_=banks[bi][:]),
                               reads=[BK[bi]], writes=["psub"])
                        for g in range(16):
                            op('dve', lambda g=g: V.max(out=a16[:, g, 0:8], in_=sub[:, g, :]),
                               reads=["psub"], writes=[f"pa16_{g}"])
                        for g in range(16):
                            op('dve', lambda g=g: V.max_index(out=ixu[:, g, 0:8], in_max=a16[:, g, 0:8],
                                                             in_values=sub[:, g, :]),
                               reads=["psub", f"pa16_{g}"], writes=[f"pixu_{g}"])
                        for g in range(16):
                            op('dve', lambda g=g: V.match_replace(out=sub2[:, g, :],
                                                                 in_to_replace=a16[:, g, 0:8],
                                                                 in_values=sub[:, g, :], imm_value=NEG),
                               reads=["psub", f"pa16_{g}"], writes=[f"psub2_{g}"])
                        for g in range(16):
                            op('dve', lambda g=g: V.max(out=a16[:, g, 8:16], in_=sub2[:, g, :]),
                               reads=[f"psub2_{g}"], writes=[f"pa16_{g}"])
                        for g in range(16):
                            op('dve', lambda g=g: V.max_index(out=ixu[:, g, 8:16], in_max=a16[:, g, 8:16],
                                                             in_values=sub2[:, g, :]),
                               reads=[f"psub2_{g}", f"pa16_{g}"], writes=[f"pixu_{g}"])
                        op('dve', lambda: V.tensor_copy(out=ixf[:], in_=ixu[:]),
                           reads=[f"pixu_{g}" for g in range(16)], writes=["pixf"])
                        in0 = AP(a16, 0, [[256, 128], [32, 8], [1, 16], [0, 16]])
                        in1 = AP(a16, 16, [[256, 128], [32, 8], [0, 16], [1, 16]])
                        candv = AP(cand, 0, [[2048, 128], [256, 8], [16, 16], [1, 16]])
                        op('dve', lambda: V.tensor_tensor(out=candv, in0=in0, in1=in1, op=ALU.add),
                           reads=[f"pa16_{g}" for g in range(16)], writes=["pcand"])
                        for h in range(8):
                            op('dve', lambda h=h: V.max(out=ts[:, h, 0:8], in_=cand[:, h, :]),
                               reads=["pcand"], writes=[f"pts_{h}"])
                        for h in range(8):
                            op('dve', lambda h=h: V.max_index(out=posu[:, h, 0:8], in_max=ts[:, h, 0:8],
                                                             in_values=cand[:, h, :]),
                               reads=["pcand", f"pts_{h}"], writes=[f"pposu_{h}"])
                        for h in range(8):
                            op('dve', lambda h=h: V.match_replace(out=cand2[:, h, :],
                                                                 in_to_replace=ts[:, h, 0:8],
                                                                 in_values=cand[:, h, :], imm_value=NEG),
                               reads=["pcand", f"pts_{h}"], writes=[f"pcand2_{h}"])
                        for h in range(8):
                            op('dve', lambda h=h: V.max(out=ts[:, h, 8:16], in_=cand2[:, h, :]),
                               reads=[f"pcand2_{h}"], writes=[f"pts_{h}"])
                        for h in range(8):
                            op('dve', lambda h=h: V.max_index(out=posu[:, h, 8:16], in_max=ts[:, h, 8:16],
                                                             in_values=cand2[:, h, :]),
                               reads=[f"pcand2_{h}", f"pts_{h}"], writes=[f"pposu_{h}"])
                        op('dve', lambda: V.tensor_copy(out=posf[:], in_=posu[:].rearrange("p a b -> p (a b)")),
                           reads=[f"pposu_{h}" for h in range(8)], writes=["pposf"])
                        posb = AP(posf, 0, [[128, 128], [1, 128], [0, 16]])
                        c16b = AP(c16, 0, [[48, 128], [0, 128], [1, 16]])
                        iob = AP(c16, 16, [[48, 128], [0, 128], [1, 16]])
                        op('dve', lambda: V.tensor_tensor(out=e0[:], in0=posb, in1=c16b, op=ALU.subtract),
                           reads=["pposf", "c16"], writes=["pe0"])
                        op('dve', lambda: V.tensor_scalar(out=e1[:], in0=e0[:], scalar1=0.0, scalar2=None,
                                                          op0=ALU.is_ge),
                           reads=["pe0"], writes=["pe1"])
                        op('dve', lambda: V.tensor_scalar(out=e0[:], in0=e0[:], scalar1=16.0, scalar2=None,
                                                          op0=ALU.is_lt),
                           reads=["pe0"], writes=["pe0"])
                        op('dve', lambda: V.tensor_tensor(out=e0[:], in0=e0[:], in1=e1[:], op=ALU.mult),
                           reads=["pe0", "pe1"], writes=["pe0"])
                        op('dve', lambda: V.tensor_tensor(out=e1[:], in0=e0[:], in1=iob, op=ALU.mult),
                           reads=["pe0", "c16"], writes=["pe1"])
                        op('dve', lambda: V.tensor_reduce(out=r0f[:], in_=e1[:], axis=AX.X, op=ALU.add),
                           reads=["pe1"], writes=["pr0f"])
                        ix0b = AP(ixf, 0, [[256, 128], [32, 8], [0, 16], [1, 16]])
                        ix1b = AP(ixf, 16, [[256, 128], [32, 8], [0, 16], [1, 16]])
                        e0v = AP(e0, 0, [[2048, 128], [256, 8], [16, 16], [1, 16]])
                        e1v = AP(e1, 0, [[2048, 128], [256, 8], [16, 16], [1, 16]])
                        op('dve', lambda: V.tensor_tensor(out=e0v, in0=e0v, in1=ix0b, op=ALU.mult),
                           reads=["pe0", "pixf"], writes=["pe0"])
                        op('dve', lambda: V.tensor_reduce(out=trin[:, 0, :], in_=e0[:], axis=AX.X, op=ALU.add),
                           reads=["pe0"], writes=["ptrin"])
                        op('dve', lambda: V.scalar_tensor_tensor(out=r1f[:], in0=r0f[:], scalar=-16.0,
                                                                 in1=posf[:], op0=ALU.mult, op1=ALU.add),
                           reads=["pr0f", "pposf"], writes=["pr1f"])
                        r1b = AP(r1f, 0, [[128, 128], [1, 128], [0, 16]])
                        op('dve', lambda: V.tensor_tensor(out=e1[:], in0=iob, in1=r1b, op=ALU.is_equal),
                           reads=["pr1f", "c16"], writes=["pe1"])
                        op('dve', lambda: V.tensor_tensor(out=e1v, in0=e1v, in1=ix1b, op=ALU.mult),
                           reads=["pe1", "pixf"], writes=["pe1"])
                        op('dve', lambda: V.tensor_reduce(out=trin[:, 1, :], in_=e1[:], axis=AX.X, op=ALU.add),
                           reads=["pe1"], writes=["ptrin"])
                        op('dve', lambda: V.tensor_copy(out=zs[:], in_=ts[:, :, 0]),
                           reads=[f"pts_{h}" for h in range(8)], writes=["pzs"])
                        mxb = AP(zs, 0, [[8, 128], [1, 8], [0, 16]])
                        op('dve', lambda: V.tensor_tensor(out=ts[:], in0=ts[:], in1=mxb, op=ALU.subtract),
                           reads=[f"pts_{h}" for h in range(8)] + ["pzs"], writes=[f"pts_{h}" for h in range(8)])
                        op('act', lambda: A_.activation(out=ts[:], in_=ts[:], func=AF.Exp),
                           reads=[f"pts_{h}" for h in range(8)], writes=[f"pts_{h}" for h in range(8)])
                        op('dve', lambda: V.tensor_reduce(out=zs[:], in_=ts[:], axis=AX.X, op=ALU.add),
                           reads=[f"pts_{h}" for h in range(8)], writes=["pzs"])
                        op('dve', lambda: V.reciprocal(out=zs[:], in_=zs[:]),
                           reads=["pzs"], writes=["pzs"])
                        zb = AP(zs, 0, [[8, 128], [1, 8], [0, 16]])
                        gout = AP(trin, 256, [[384, 128], [16, 8], [1, 16]])
                        op('dve', lambda: V.tensor_tensor(out=gout, in0=ts[:], in1=zb, op=ALU.mult),
                           reads=[f"pts_{h}" for h in range(8)] + ["pzs"], writes=["ptrin"])
                        pb = bank_bf(7)
                        for w in range(3):
                            op('pe', lambda w=w, pb=pb: T_.transpose(out=pb[:, w * 128:(w + 1) * 128],
                                                                     in_=trin[:, w, :], identity=identb[:]),
                               reads=["ptrin", "identb"], writes=[BK[7]])
                        op('act', lambda pb=pb: A_.copy(out=trT[:].rearrange("p a b -> p (a b)"),
                                                        in_=pb[:, 0:384]),
                           reads=[BK[7]], writes=[f"ptrT{par}{tt}"])

                    for tt in range(2):
                        tile_body(tt)

                def r2(blk):
                    par = blk % 2
                    for tt in range(2):
                        trT = trTs[par][tt]
                        for q8 in range(16):
                            lb = q8 % 2
                            t0 = q8 * 8
                            for tl in range(8):
                                t = t0 + tl
                                op('dve', lambda lb=lb, tl=tl, t=t: V.tensor_scalar(
                                    out=Lc[lb][:, tl, :], in0=iota128[:], scalar1=trT[:, 0, t:t + 1], scalar2=None,
                                    op0=ALU.is_equal),
                                   reads=["iota128", f"ptrT{par}{tt}"], writes=[f"pLc{lb}_{tl}"])
                                op('dve', lambda lb=lb, tl=tl, t=t: V.tensor_scalar(
                                    out=Rc[lb][:, tl, :], in0=iota128[:], scalar1=trT[:, 1, t:t + 1],
                                    scalar2=trT[:, 2, t:t + 1], op0=ALU.is_equal, op1=ALU.mult),
                                   reads=["iota128", f"ptrT{par}{tt}"], writes=[f"pRc{lb}_{tl}"])
                            for q in range(2):
                                bi = 4 + ((q8 * 2 + q) % 2)
                                for t4 in range(4):
                                    tl = q * 4 + t4
                                    op('pe', lambda lb=lb, tl=tl, t4=t4, bi=bi: T_.matmul(
                                        banks[bi][:, t4 * 128:(t4 + 1) * 128],
                                        lhsT=Rc[lb][:, tl, :], rhs=Lc[lb][:, tl, :],
                                        start=True, stop=True),
                                       reads=[f"pRc{lb}_{tl}", f"pLc{lb}_{tl}"], writes=[BK[bi]])
                                tg = tt * 128 + t0 + q * 4
                                op('act', lambda bi=bi, tg=tg: A_.copy(
                                    out=WT2[:, tg:tg + 4, :].rearrange("p a b -> p (a b)"),
                                    in_=banks[bi][:]),
                                   reads=[BK[bi]], writes=["WT2"])

                def chunks(blk, filler):
                    par = blk % 2
                    xnT = xnTs[par]
                    def ld(c):
                        b = c % NSB
                        dma('sp', utc[b][:], ut_s.ap()[c], reads=["ut_s"], writes=[f"utc{b}"], slot=f"utc{b}")
                        dma('sp', vc[b][:], v_s.ap()[c], reads=["v_s"], writes=[f"vc{b}"], slot=f"vc{b}")

                    def emitA(c):
                        b = c % NSB
                        bi = 4 + c % 3
                        for kc in range(8):
                            op('pe', lambda kc=kc, b=b, bi=bi: T_.matmul(
                                banks[bi][:, 0:T], lhsT=utc[b][:, kc * 128:(kc + 1) * 128],
                                rhs=xnT[:, kc, :], start=(kc == 0), stop=(kc == 7)),
                               reads=[f"utc{b}", f"xnT{par}"], writes=[BK[bi]])

                    def emitB(c):
                        b = c % NSB
                        g2_ = c % 3
                        bi = 4 + c % 3
                        op('act', lambda g2_=g2_, bi=bi: A_.activation(out=gact[g2_][:], in_=banks[bi][:, 0:T],
                                                                     func=AF.Gelu),
                           reads=[BK[bi]], writes=[f"pga{g2_}"])
                        wv = AP(WT2, c, [[T * 128, 128], [128, T]])
                        op('pool', lambda g2_=g2_, wv=wv: P_.tensor_tensor(out=gw[g2_][:], in0=gact[g2_][:], in1=wv,
                                                                       op=ALU.mult),
                           reads=[f"pga{g2_}", "WT2"], writes=[f"pgw{g2_}"])
                        for tt in range(2):
                            for hf in range(2):
                                bo = tt * 2 + hf
                                op('pe', lambda b=b, g2_=g2_, tt=tt, hf=hf, bo=bo, c=c: T_.matmul(
                                    banks[bo][:], lhsT=gw[g2_][:, tt * 128:(tt + 1) * 128],
                                    rhs=vc[b][:, hf * 512:(hf + 1) * 512],
                                    start=(c == 0), stop=(c == 127)),
                                   reads=[f"pgw{g2_}", f"vc{b}"], writes=[BK[bo]])

                    for c in range(min(NSB - 1, 128)):
                        ld(c)
                    emitA(0)
                    emitA(1)
                    for c in range(128):
                        filler()
                        if c + NSB - 1 < 128:
                            ld(c + NSB - 1)
                        if c + 2 < 128:
                            emitA(c + 2)
                        emitB(c)

                def epi(blk):
                    par = blk % 2
                    hb = hbs[par]
                    for tt in range(2):
                        tile = blk * 2 + tt
                        for hf in range(2):
                            bo = tt * 2 + hf
                            op('dve', lambda tt=tt, hf=hf, bo=bo: V.tensor_tensor(
                                out=hb[tt][:, hf * 512:(hf + 1) * 512], in0=banks[bo][:],
                                in1=hb[tt][:, hf * 512:(hf + 1) * 512], op=ALU.add),
                               reads=[BK[bo], f"hb{par}{tt}"], writes=[f"hb{par}{tt}"])
                        dma('pool', y_d.ap()[tile * 128:(tile + 1) * 128, :], hb[tt][:],
                            reads=[f"hb{par}{tt}"], writes=["y"], slot=f"y{tt}")


                r1(0, op, dma)
                r2(0)
                for blk in range(NBLK):
                    dq = Deferred()
                    if blk + 1 < NBLK:
                        r1(blk + 1, dq.op, dq.dma)
                    per = (len(dq.q) + 99) // 100
                    chunks(blk, lambda: dq.run(per))
                    dq.run(10 ** 9)
                    if blk + 1 < NBLK:
                        r2(blk + 1)
                    epi(blk)
                S_.barrier()


        def mixer_a():
            CH = [(0, 512), (512, 1024), (1024, 1536), (1536, 2048), (2048, 2120), (2120, 2632),
                  (2632, 3144), (3144, 3656), (3656, 4168), (4168, 4680)]
            with ExitStack() as st:
                winb = sb(st, "winb", [128, 8, 4680], BF16)
                wbpb = sb(st, "wbpb", [128, 4, D], BF16)
                poolwb = sb(st, "poolwb", [128, 4, 128], BF16)
                bandt = sb(st, "bandt", [128, 3, 4, 128], F32)
                pscale = sb(st, "pscale", [128, 4], F32)
                gqk = sb(st, "gqk", [128, 2, 64], F32)
                ropeall = sb(st, "ropeall", [128, TPS, 16], F32)
                with ExitStack() as st2:
                    wst = [sb(st2, f"wst{i}", [128, 4680], F32) for i in range(2)]
                    for kc in range(8):
                        b = kc % 2
                        dma('sp', wst[b][:], win_d.ap()[kc * 128:(kc + 1) * 128, :], writes=[f"wst{b}"],
                            slot=f"wst{b}")
                        op('dve' if b == 0 else 'pool',
                           (lambda kc=kc, b=b: V.tensor_copy(out=winb[:, kc, :], in_=wst[b][:])) if b == 0 else
                           (lambda kc=kc, b=b: P_.tensor_copy(out=winb[:, kc, :], in_=wst[b][:])),
                           reads=[f"wst{b}"], writes=["winb"])
                    dma('sp', wst[0][:, 0:4096].rearrange("p (a b) -> p a b", a=4),
                        wbp_d.ap().rearrange("(a p) c -> p a c", p=128), writes=["wst0"], slot="wst0")
                    op('dve', lambda: V.tensor_copy(out=wbpb[:].rearrange("p a b -> p (a b)"), in_=wst[0][:, 0:4096]),
                       reads=["wst0"], writes=["wbpb"])
                    dma('sp', wst[1][:, 0:512].rearrange("p (a b) -> p a b", a=4), poolw_d.ap(),
                        writes=["wst1"], slot="wst1")
                    op('dve', lambda: V.tensor_copy(out=poolwb[:].rearrange("p a b -> p (a b)"), in_=wst[1][:, 0:512]),
                       reads=["wst1"], writes=["poolwb"])
                    dma('sp', bandt[:], band_d.ap().rearrange("k s g t -> s k g t"), writes=["bandt"], slot="c5")
                    dma('sp', pscale[:], pscale_d.ap(), writes=["pscale"], slot="c6")
                    dma('sp', gqk[:], gqk_d.ap(), writes=["gqk"], slot="c7")
                    dma('sp', ropeall[:].rearrange("p i (a b) -> p i a b", a=2),
                        rope_d.ap().rearrange("i p a b -> p i a b"), writes=["ropeall"], slot="c8")
                    S_.barrier()

                xt = [sb(st, f"xt{i}", [128, D], F32) for i in range(2)]
                junk = sb(st, "ajunk", [128, D], BF16)
                xnb = sb(st, "axnb", [128, D], BF16)
                ssq = sb(st, "assq", [128, 1], F32)
                rstd = sb(st, "arstd", [128, 1], F32)
                xnT = sb(st, "axnT", [128, 8, 128], BF16)
                pj = sb(st, "apj", [128, 2176], F32)
                sq = sb(st, "asq", [128, 512], F32)
                s8 = sb(st, "as8", [128, 8], F32)
                pp = [sb(st, f"app{i}", [128, 512], F32) for i in range(2)]
                qkb = sb(st, "aqkb", [128, 2176], BF16)
                vaug = sb(st, "avaug", [128, 8, 65], BF16)
                qTz = sb(st, "aqTz", [128, 4, 2, 128], BF16)
                qiTz = sb(st, "aqiTz", [128, 4, 2, 128], BF16)
                kT = sb(st, "akT", [128, 4, 128], BF16)
                kiT = sb(st, "akiT", [128, 128], BF16)
                wis = sb(st, "awis", [128, 8], F32)
                gab = sb(st, "agab", [128, D], BF16)
                gpb = sb(st, "agpb", [128, D], BF16)
                zpb = sb(st, "azpb", [128, D], BF16)
                pooledT = sb(st, "apooledT", [128, 4, 128], BF16)
                mixedT = sb(st, "amixedT", [128, 4, 128], BF16)
                rt = sb(st, "art", [128, 4, 34, 8], F32)
                op('dve', lambda: V.memset(vaug[:], 1.0), writes=["avaug"])
                op('dve', lambda: V.memset(qTz[:], 0.0), writes=["aqTz"])
                op('dve', lambda: V.memset(qiTz[:], 0.0), writes=["aqiTz"])
                op('dve', lambda: V.memset(pj[:], 0.0), writes=["apj"])

                pq = Deferred()
                if "peer" in phases:
                    prep_ops(st, pq.op, pq.dma)
                pper = (len(pq.q) + NTILE - 1) // NTILE
                for t in range(NTILE):
                    pq.run(pper)
                    b_, i = divmod(t, TPS)
                    xb = t % 2
                    dma('sp', xt[xb][:], x_d.ap()[t * 128:(t + 1) * 128, :], writes=[f"xt{xb}"], slot=f"xt{xb}")
                    norm_transpose("a", xt[xb], f"xt{xb}", junk, ssq, rstd, xnb, g1, "g1",
                                   lambda: xnT[:], "axnT", 7)
                    pc = pp[i % 2]
                    pck = f"app{i % 2}"
                    for ci, (c0, c1) in enumerate(CH):
                        n = c1 - c0
                        bi = ci % 4
                        for kc in range(8):
                            op('pe', lambda kc=kc, bi=bi, c0=c0, c1=c1, n=n: T_.matmul(
                                banks[bi][:, 0:n], lhsT=xnT[:, kc, :], rhs=winb[:, kc, c0:c1],
                                start=(kc == 0), stop=(kc == 7)),
                               reads=["axnT", "winb"], writes=[BK[bi]])
                        if ci in (0, 1):
                            op('act', lambda bi=bi: A_.activation(out=sq[:], in_=banks[bi][:], func=AF.Square),
                               reads=[BK[bi]], writes=["asq"])
                            op('dve', lambda: V.tensor_reduce(out=s8[:], in_=sq[:].rearrange("p (a b) -> p a b", a=8),
                                                              axis=AX.X, op=ALU.add),
                               reads=["asq"], writes=["as8"])
                            op('dve', lambda: V.tensor_scalar(out=s8[:], in0=s8[:], scalar1=1.0 / 64, scalar2=EPS,
                                                              op0=ALU.mult, op1=ALU.add),
                               reads=["as8"], writes=["as8"])
                            op('act', lambda: A_.activation(out=s8[:], in_=s8[:], func=AF.Sqrt),
                               reads=["as8"], writes=["as8"])
                            op('dve', lambda: V.reciprocal(out=s8[:], in_=s8[:]), reads=["as8"], writes=["as8"])
                            pjv = AP(pj, c0, [[2176, 128], [64, 8], [1, 64]])
                            psv = AP(banks[bi], 0, [[512, 128], [64, 8], [1, 64]])
                            s8b = AP(s8, 0, [[8, 128], [1, 8], [0, 64]])
                            gb = AP(gqk, ci * 64, [[128, 128], [0, 8], [1, 64]])
                            op('dve', lambda pjv=pjv, psv=psv, s8b=s8b: V.tensor_tensor(out=pjv, in0=psv, in1=s8b,
                                                                                    op=ALU.mult),
                               reads=[BK[bi], "as8"], writes=["apj"])
                            op('dve', lambda pjv=pjv, gb=gb: V.tensor_tensor(out=pjv, in0=pjv, in1=gb, op=ALU.mult),
                               reads=["apj", "gqk"], writes=["apj"])
                        elif ci == 2:
                            psv = AP(banks[bi], 0, [[512, 128], [64, 8], [1, 64]])
                            op('act', lambda psv=psv: A_.copy(out=vaug[:, :, 0:64], in_=psv),
                               reads=[BK[bi]], writes=["avaug"])
                            dma('pool', v2_s.ap()[b_, i * 128:(i + 1) * 128, :],
                                vaug[:].rearrange("p a b -> p (a b)"), reads=["avaug"], writes=["v2_s"], slot="sv")
                        elif ci in (3, 4):
                            op('act', lambda bi=bi, c0=c0, c1=c1, n=n: A_.copy(out=pj[:, c0:c1], in_=banks[bi][:, 0:n]),
                               reads=[BK[bi]], writes=["apj"])
                        elif ci == 5:
                            op('act', lambda bi=bi, pc=pc: A_.copy(out=pc[:], in_=banks[bi][:]),
                               reads=[BK[bi]], writes=[pck])
                        else:
                            dst = gab if ci in (6, 7) else gpb
                            dk = "agab" if ci in (6, 7) else "agpb"
                            hf = ci % 2
                            op('act', lambda bi=bi, dst=dst, hf=hf: A_.activation(
                                out=dst[:, hf * 512:(hf + 1) * 512], in_=banks[bi][:], func=AF.Sigmoid),
                               reads=[BK[bi]], writes=[dk])
                    x1 = AP(pj, 0, [[2176, 128], [64, 33], [1, 8]])
                    x2 = AP(pj, 8, [[2176, 128], [64, 33], [1, 8]])
                    cosb = AP(ropeall, i * 16, [[TPS * 16, 128], [0, 33], [1, 8]])
                    sinb = AP(ropeall, i * 16 + 8, [[TPS * 16, 128], [0, 33], [1, 8]])
                    rts = [AP(rt, k * 272, [[1088, 128], [8, 33], [1, 8]]) for k in range(4)]
                    for k, (a_, b2) in enumerate(((x1, cosb), (x2, sinb), (x2, cosb), (x1, sinb))):
                        op('dve', lambda k=k, a_=a_, b2=b2: V.tensor_tensor(out=rts[k], in0=a_, in1=b2, op=ALU.mult),
                           reads=["apj", "ropeall"], writes=[f"art{k}"])
                    op('dve', lambda: V.tensor_tensor(out=x1, in0=rts[0], in1=rts[1], op=ALU.subtract),
                       reads=["art0", "art1"], writes=["apj"])
                    op('dve', lambda: V.tensor_tensor(out=x2, in0=rts[2], in1=rts[3], op=ALU.add),
                       reads=["art2", "art3"], writes=["apj"])
                    op('act', lambda: A_.copy(out=qkb[:, 0:1024], in_=pj[:, 0:1024]), reads=["apj"], writes=["aqkb"])
                    op('act', lambda: A_.copy(out=qkb[:, 1536:2112], in_=pj[:, 1536:2112]), reads=["apj"],
                       writes=["aqkb"])
                    op('act', lambda: A_.copy(out=qkb[:, 2112:2176], in_=pj[:, 2048:2112]), reads=["apj"],
                       writes=["aqkb"])
                    op('dve', lambda: V.tensor_scalar(out=wis[:], in0=pj[:, 2112:2120], scalar1=(8 ** -0.5) * 0.125,
                                                      scalar2=None, op0=ALU.mult),
                       reads=["apj"], writes=["awis"])
                    dma('pool', wi_s.ap()[t], wis[:], reads=["awis"], writes=["wi_s"], slot="swi")
                    p4, p5 = bank_bf(4), bank_bf(5)
                    for c in range(8):
                        op('pe', lambda c=c: T_.transpose(out=p4[:, c * 128:(c + 1) * 128],
                                                          in_=qkb[:, c * 128:(c + 1) * 128], identity=identb[:]),
                           reads=["aqkb", "identb"], writes=[BK[4]])
                    for c in range(5):
                        op('pe', lambda c=c: T_.transpose(out=p5[:, c * 128:(c + 1) * 128],
                                                          in_=qkb[:, 1536 + c * 128:1536 + (c + 1) * 128],
                                                          identity=identb[:]),
                           reads=["aqkb", "identb"], writes=[BK[5]])
                    for par in range(2):
                        r0_, r1_ = par * 64, par * 64 + 64
                        src = AP(p4.tensor, p4.offset + r0_ * p4.ap[0][0], [[p4.ap[0][0], 64], [128, 4], [1, 128]])
                        dst = AP(qTz, r0_ * 1024 + par * 128, [[1024, 64], [256, 4], [1, 128]])
                        op('act', lambda src=src, dst=dst: A_.copy(out=dst, in_=src), reads=[BK[4]], writes=["aqTz"])
                        src = AP(p5.tensor, p5.offset + r0_ * p5.ap[0][0], [[p5.ap[0][0], 64], [128, 4], [1, 128]])
                        dst = AP(qiTz, r0_ * 1024 + par * 128, [[1024, 64], [256, 4], [1, 128]])
                        op('dve', lambda src=src, dst=dst: V.tensor_copy(out=dst, in_=src), reads=[BK[5]],
                           writes=["aqiTz"])
                    op('act', lambda: A_.copy(out=kT[:].rearrange("p a b -> p (a b)"), in_=p4[:, 512:1024]),
                       reads=[BK[4]], writes=["akT"])
                    op('dve', lambda: V.tensor_copy(out=kiT[:], in_=p5[:, 512:640]), reads=[BK[5]], writes=["akiT"])
                    dma('pool', qT_s.ap()[t], qTz[:].rearrange("p a b c -> p (a b c)"), reads=["aqTz"],
                        writes=["qT_s"], slot="sq")
                    dma('pool', qiT_s.ap()[t], qiTz[:].rearrange("p a b c -> p (a b c)"), reads=["aqiTz"],
                        writes=["qiT_s"], slot="sqi")
                    dma('pool', kT_s.ap()[b_, :, :, i * 128:(i + 1) * 128], kT[:], reads=["akT"],
                        writes=["kT_s"], slot="sk")
                    dma('pool', kiT_s.ap()[b_, :, i * 128:(i + 1) * 128], kiT[:], reads=["akiT"],
                        writes=["kiT_s"], slot="ski")
                    for g in range(4):
                        op('pe', lambda g=g, pc=pc, i=i: T_.matmul(
                            banks[6][:, g * 128:(g + 1) * 128], lhsT=pc[:, g * 128:(g + 1) * 128],
                            rhs=bandt[:, 0 if i == 0 else 1, g, :], start=True, stop=(i == 0)),
                           reads=[pck, "bandt"], writes=[BK[6]])
                        if i > 0:
                            pv = pp[(i + 1) % 2]
                            op('pe', lambda g=g, pv=pv: T_.matmul(
                                banks[6][:, g * 128:(g + 1) * 128], lhsT=pv[:, g * 128:(g + 1) * 128],
                                rhs=bandt[:, 2, g, :], start=False, stop=True),
                               reads=[f"app{(i + 1) % 2}", "bandt"], writes=[BK[6]])
                    op('act', lambda: A_.copy(out=pooledT[:].rearrange("p a b -> p (a b)"), in_=banks[6][:]),
                       reads=[BK[6]], writes=["apooledT"])
                    for g in range(4):
                        op('pe', lambda g=g: T_.matmul(banks[6][:, g * 128:(g + 1) * 128], lhsT=poolwb[:, g, :],
                                                       rhs=pooledT[:, g, :], start=True, stop=True),
                           reads=["poolwb", "apooledT"], writes=[BK[6]])
                    psb = AP(pscale, 0, [[4, 128], [1, 4], [0, 128]])
                    op('dve', lambda: V.tensor_tensor(out=mixedT[:], in0=banks[6][:].rearrange("p (a b) -> p a b", a=4),
                                                      in1=psb, op=ALU.mult),
                       reads=[BK[6], "pscale"], writes=["amixedT"])
                    for hf in range(2):
                        bi = hf
                        for g in range(4):
                            op('pe', lambda g=g, hf=hf, bi=bi: T_.matmul(
                                banks[bi][:], lhsT=mixedT[:, g, :], rhs=wbpb[:, g, hf * 512:(hf + 1) * 512],
                                start=(g == 0), stop=(g == 3)),
                               reads=["amixedT", "wbpb"], writes=[BK[bi]])
                        op('dve', lambda hf=hf, bi=bi: V.tensor_tensor(
                            out=zpb[:, hf * 512:(hf + 1) * 512], in0=banks[bi][:],
                            in1=gpb[:, hf * 512:(hf + 1) * 512], op=ALU.mult),
                           reads=[BK[bi], "agpb"], writes=["azpb"])
                    dma('pool', zp_s.ap()[t], zpb[:], reads=["azpb"], writes=["zp_s"], slot="szp")
                    dma('pool', ga_s.ap()[t], gab[:], reads=["agab"], writes=["ga_s"], slot="sga")
                pq.run(10 ** 9)
                S_.barrier()

        def mixer_b():
            NIT = 17
            TOPK = min(256, S // 4)
            with ExitStack() as st:
                KT = sb(st, "bKT", [128, 4, S], BF16)
                Va = sb(st, "bVa", [128, TPS, 520], BF16)
                kiT = sb(st, "bkiT", [128, S], BF16)
                wbab = sb(st, "bwbab", [128, 4, D], BF16)
                woutb = sb(st, "bwoutb", [128, 8, D], BF16)
                bmask = sb(st, "bbmask", [128, 128], F32)
                thrc = sb(st, "bthrc", [128, 1], F32)
                cpow = sb(st, "bcpow", [128, NIT + 1], F32)
                score = [sb(st, f"bscore{i}", [128, S], F32) for i in range(2)]
                mneg = [sb(st, f"bmneg{i}", [128, S], BF16) for i in range(2)]
                cj = sb(st, "bcj", [128, S], BF16)
                ident4 = sb(st, "bident4", [128, 4, 128], BF16)
                with ExitStack() as st2:
                    wst = sb(st2, "bwst", [128, 8, D], F32)
                    dma('sp', wst[:], wout_d.ap().rearrange("(a p) c -> p a c", p=128), writes=["bwst"], slot="c5")
                    op('dve', lambda: V.tensor_copy(out=woutb[:], in_=wst[:]), reads=["bwst"], writes=["bwoutb"])
                    dma('sp', wst[:, 0:4, :], wba_d.ap().rearrange("(a p) c -> p a c", p=128), reads=[],
                        writes=["bwst"], slot="c5")
                    op('dve', lambda: V.tensor_copy(out=wbab[:], in_=wst[:, 0:4, :]), reads=["bwst"], writes=["bwbab"])
                    dma('sp', bmask[:], bmask_d.ap(), writes=["bbmask"], slot="c6")
                    op('dve', lambda: V.memset(thrc[:], -1.0e29), writes=["bthrc"])
                    for k in range(4):
                        op('dve', lambda k=k: V.tensor_copy(out=ident4[:, k, :], in_=identb[:]), reads=["identb"], writes=["bident4"])
                    for k in range(NIT + 1):
                        op('dve', lambda k=k: V.memset(cpow[:, k:k + 1], 0.5 ** k), writes=["bcpow"])
                    S_.barrier()
                qTz = [sb(st, f"bqTz{i}", [128, 4, 2, 128], BF16) for i in range(2)]
                qiTz = [sb(st, f"bqiTz{i}", [128, 4, 2, 128], BF16) for i in range(2)]
                wis = [sb(st, f"bwis{i}", [128, 8], F32) for i in range(2)]
                gab = [sb(st, f"bgab{i}", [128, D], BF16) for i in range(2)]
                zpb = [sb(st, f"bzpb{i}", [128, D], BF16) for i in range(2)]
                xt = [sb(st, f"bxt{i}", [128, D], F32) for i in range(2)]
                dg = [sb(st, f"bdg{i}", [128, 8, 128], BF16) for i in range(2)]
                rl = [sb(st, f"brl{i}", [128, 512], BF16) for i in range(2)]
                PT = [sb(st, f"bPT{i}", [128, 512], BF16) for i in range(3)]
                lo = sb(st, "blo", [128, 1], F32)
                w0 = sb(st, "bw0", [128, 1], F32)
                hw2 = sb(st, "bhw2", [128, NIT + 1], F32)
                mid = [sb(st, f"bmid{i}", [128, 1], F32) for i in range(2)]
                cnt = sb(st, "bcnt", [128, 1], F32)
                stp = sb(st, "bstp", [128, 1], F32)
                rsum = sb(st, "brsum", [128, 8], F32)
                attn = sb(st, "battn", [128, 512], BF16)
                attnT = sb(st, "battnT", [128, 4, 128], BF16)
                zf = sb(st, "bzf", [128, D], BF16)
                zb = sb(st, "bzb", [128, D], BF16)
                zT = sb(st, "bzT", [128, 8, 128], BF16)
                HB = {("0", 0): (0, 0), ("0", 1): (0, 256), ("1", 0): (1, 0), ("1", 1): (1, 256)}

                def load_seq_ki(b_):
                    dma('sp', kiT[:], kiT_s.ap()[b_], reads=["kiT_s"], writes=["bkiT"], slot="lki")

                def load_seq_kv(b_):
                    dma('sp', KT[:], kT_s.ap()[b_], reads=["kT_s"], writes=["bKT"], slot="lk")
                    for q in range(0, TPS, 8):
                        q1 = min(TPS, q + 8)
                        dma('sp', Va[:, q:q1, :],
                            v2_s.ap()[b_, q * 128:q1 * 128, :].rearrange("(a p) c -> p a c", p=128),
                            reads=["v2_s"], writes=["bVa"], slot="lv")

                def s1_load_diag(t):
                    b_, i = divmod(t, TPS)
                    L = 128 * (i + 1)
                    d = t % 2
                    sc, sck = score[d], f"bscore{d}"
                    dma('sp', qiTz[d][:].rearrange("p a b c -> p (a b c)"), qiT_s.ap()[t], reads=["qiT_s"],
                        writes=[f"bqiTz{d}"], slot=f"lqi{d}")
                    dma('sp', wis[d][:], wi_s.ap()[t], reads=["wi_s"], writes=[f"bwis{d}"], slot=f"lwi{d}")
                    for h in range(8):
                        op('dve', lambda h=h, d=d: V.tensor_scalar(out=dg[d][:, h, :], in0=identb[:],
                                                                   scalar1=wis[d][:, h:h + 1], scalar2=None,
                                                                   op0=ALU.mult),
                           reads=["identb", f"bwis{d}"], writes=[f"bdg{d}"])

                def s1_index(t):
                    b_, i = divmod(t, TPS)
                    L = 128 * (i + 1)
                    d = t % 2
                    sc, sck = score[d], f"bscore{d}"
                    nch = (L + 511) // 512
                    LB = (0, 5)
                    for c in range(nch):
                        n = min(512, L - 512 * c)

                        def lg(h, c=c, n=n):
                            bi = LB[h % 2]
                            op('pe', lambda: T_.matmul(
                                banks[bi][:, 0:n], lhsT=qiTz[d][:, h // 2, h % 2, :],
                                rhs=kiT[:, 512 * c:512 * c + n], start=True, stop=True),
                               reads=[f"bqiTz{d}", "bkiT"], writes=[BK[bi]])
                            op('act', lambda: A_.activation(out=rl[h % 2][:, 0:n], in_=banks[bi][:, 0:n],
                                                            func=AF.Relu),
                               reads=[BK[bi]], writes=[f"brl{h % 2}"])

                        def ac(h, n=n):
                            op('pe', lambda: T_.matmul(
                                banks[1][:, 0:n], lhsT=dg[d][:, h, :], rhs=rl[h % 2][:, 0:n],
                                start=(h == 0), stop=(h == 7)),
                               reads=[f"bdg{d}", f"brl{h % 2}"], writes=[BK[1]])

                        lg(0)
                        for h in range(8):
                            if h + 1 < 8:
                                lg(h + 1)
                            ac(h)
                        op('act', lambda c=c, n=n, sc=sc: A_.copy(out=sc[:, 512 * c:512 * c + n],
                                                                 in_=banks[1][:, 0:n]),
                           reads=[BK[1]], writes=[sck])

                def s1_bisect(t):
                    b_, i = divmod(t, TPS)
                    L = 128 * (i + 1)
                    d = t % 2
                    sc, sck = score[d], f"bscore{d}"
                    op('dve', lambda L=L, sc=sc: V.tensor_tensor(out=sc[:, L - 128:L], in0=sc[:, L - 128:L],
                                                                 in1=bmask[:], op=ALU.add),
                       reads=[sck, "bbmask"], writes=[sck])
                    if 128 * i + 64 > TOPK:
                        op('dve', lambda L=L, sc=sc: V.tensor_reduce(out=lo[:], in_=sc[:, 0:TOPK], axis=AX.X,
                                                                     op=ALU.min),
                           reads=[sck], writes=["blo"])
                        op('dve', lambda L=L, sc=sc: V.tensor_reduce(out=w0[:], in_=sc[:, 0:L], axis=AX.X, op=ALU.max),
                           reads=[sck], writes=["bw0"])
                        op('dve', lambda: V.tensor_tensor(out=w0[:], in0=w0[:], in1=lo[:], op=ALU.subtract),
                           reads=["bw0", "blo"], writes=["bw0"])
                        w0b = AP(w0, 0, [[1, 128], [0, NIT + 1]])
                        op('dve', lambda w0b=w0b: V.tensor_tensor(out=hw2[:], in0=cpow[:], in1=w0b, op=ALU.mult),
                           reads=["bw0", "bcpow"], writes=["bhw2"])
                        op('dve', lambda: V.scalar_tensor_tensor(out=mid[0][:], in0=w0[:], scalar=0.5, in1=lo[:],
                                                                 op0=ALU.mult, op1=ALU.add),
                           reads=["bw0", "blo"], writes=["bmid0"])
                        nit = min(NIT, max(12, int(np.ceil(np.log2(20.0 * L)))))
                        for it in range(nit):
                            mi, mo = it % 2, (it + 1) % 2
                            op('dve', lambda L=L, sc=sc, mi=mi: V.tensor_scalar(
                                out=cj[:, 0:L], in0=sc[:, 0:L], scalar1=mid[mi][:, 0:1], scalar2=None,
                                op0=ALU.is_ge, op1=ALU.add, accum_out=cnt[:]),
                               reads=[sck, f"bmid{mi}"], writes=["bcj", "bcnt"])
                            last = (it == nit - 1)
                            op('dve', lambda last=last: V.tensor_scalar(
                                out=stp[:], in0=cnt[:], scalar1=TOPK - 0.5, scalar2=(1.0 if last else 0.5),
                                op0=ALU.is_ge, op1=ALU.subtract),
                               reads=["bcnt"], writes=["bstp"])
                            k = nit if last else it + 1
                            op('dve', lambda k=k, mi=mi, mo=mo: V.scalar_tensor_tensor(
                                out=mid[mo][:], in0=stp[:], scalar=hw2[:, k:k + 1], in1=mid[mi][:],
                                op0=ALU.mult, op1=ALU.add),
                               reads=["bstp", "bhw2", f"bmid{mi}"], writes=[f"bmid{mo}"])
                        thr, thrk = mid[nit % 2], f"bmid{nit % 2}"
                    else:
                        thr, thrk = thrc, "bthrc"
                    op('dve', lambda L=L, thr=thr, sc=sc, d=d: V.tensor_scalar(
                        out=mneg[d][:, 0:L], in0=sc[:, 0:L], scalar1=thr[:, 0:1], scalar2=-30000.0,
                        op0=ALU.is_lt, op1=ALU.mult),
                       reads=[sck, thrk], writes=[f"bmneg{d}"])


                def s2_load(t):
                    b_, i = divmod(t, TPS)
                    L = 128 * (i + 1)
                    d = t % 2
                    sc, sck = score[d], f"bscore{d}"
                    dma('sp', qTz[d][:].rearrange("p a b c -> p (a b c)"), qT_s.ap()[t], reads=["qT_s"],
                        writes=[f"bqTz{d}"], slot=f"lq{d}")
                    dma('sp', gab[d][:], ga_s.ap()[t], reads=["ga_s"], writes=[f"bgab{d}"], slot=f"lga{d}")
                    dma('sp', zpb[d][:], zp_s.ap()[t], reads=["zp_s"], writes=[f"bzpb{d}"], slot=f"lzp{d}")
                    dma('sp', xt[d][:], x_d.ap()[t * 128:(t + 1) * 128, :], writes=[f"bxt{d}"], slot=f"lx{d}")

                def stage2(t):
                    b_, i = divmod(t, TPS)
                    d = t % 2
                    SB3 = (2, 3, 4)
                    units = [(kc, half) for kc in range(i + 1) for half in range(2)]

                    def emitS(u):
                        kc, half = units[u]
                        bi = SB3[u % 3]
                        op('pe', lambda: T_.matmul(
                            banks[bi][:], lhsT=mneg[d][:, kc * 128:(kc + 1) * 128],
                            rhs=ident4[:].rearrange("p a b -> p (a b)"), start=True, stop=False),
                           reads=[f"bmneg{d}", "bident4"], writes=[BK[bi]])
                        for hp2 in range(2):
                            hp = half * 2 + hp2
                            op('pe', lambda hp=hp, hp2=hp2: T_.matmul(
                                banks[bi][:, hp2 * 256:(hp2 + 1) * 256], lhsT=KT[:, hp, kc * 128:(kc + 1) * 128],
                                rhs=qTz[d][:, hp, :, :].rearrange("p a b -> p (a b)"), start=False, stop=(hp2 == 1)),
                               reads=["bKT", f"bqTz{d}"], writes=[BK[bi]])

                    def emitP(u):
                        kc, half = units[u]
                        bi = SB3[u % 3]
                        pt = PT[u % 3]
                        ptk = f"bPT{u % 3}"
                        op('act', lambda: A_.activation(out=pt[:], in_=banks[bi][:], func=AF.Exp, scale=0.125),
                           reads=[BK[bi]], writes=[ptk])
                        for j in range(4):
                            h = half * 4 + j
                            bo = 6 + half
                            op('pe', lambda h=h, j=j, bo=bo: T_.matmul(
                                banks[bo][:, j * 65:j * 65 + 65], lhsT=pt[:, j * 128:(j + 1) * 128],
                                rhs=Va[:, kc, h * 65:(h + 1) * 65], start=(kc == 0 and j == 0),
                                stop=(kc == i and j == 3), skip_group_check=True),
                               reads=[ptk, "bVa"], writes=[BK[bo]])

                    emitS(0)
                    for u in range(len(units)):
                        if u + 1 < len(units):
                            emitS(u + 1)
                        emitP(u)
                    for hf in range(2):
                        ov = AP(banks[6 + hf], 0, [[512, 128], [65, 4], [1, 64]])
                        sv = AP(banks[6 + hf], 64, [[512, 128], [65, 4], [1, 1]])
                        op('dve', lambda hf=hf, sv=sv: V.reciprocal(
                            out=rsum[:, hf * 4:(hf + 1) * 4].rearrange("p (a b) -> p a b", b=1), in_=sv),
                           reads=[BK[6 + hf]], writes=["brsum"])
                        rb = AP(rsum, hf * 4, [[8, 128], [1, 4], [0, 64]])
                        av = AP(attn, hf * 256, [[512, 128], [64, 4], [1, 64]])
                        op('dve', lambda ov=ov, rb=rb, av=av: V.tensor_tensor(out=av, in0=ov, in1=rb, op=ALU.mult),
                           reads=[BK[6 + hf], "brsum"], writes=["battn"])
                    p0 = bank_bf(6)
                    for c in range(4):
                        op('pe', lambda c=c: T_.transpose(out=p0[:, c * 128:(c + 1) * 128],
                                                          in_=attn[:, c * 128:(c + 1) * 128], identity=identb[:]),
                           reads=["battn", "identb"], writes=[BK[6]])
                    op('act', lambda: A_.copy(out=attnT[:].rearrange("p a b -> p (a b)"), in_=p0[:, 0:512]),
                       reads=[BK[6]], writes=["battnT"])
                    for hf in range(2):
                        bi = 2 + hf
                        for c in range(4):
                            op('pe', lambda c=c, hf=hf, bi=bi: T_.matmul(
                                banks[bi][:], lhsT=attnT[:, c, :], rhs=wbab[:, c, hf * 512:(hf + 1) * 512],
                                start=(c == 0), stop=(c == 3)),
                               reads=["battnT", "bwbab"], writes=[BK[bi]])
                        op('dve', lambda hf=hf, bi=bi, d=d: V.tensor_tensor(
                            out=zf[:, hf * 512:(hf + 1) * 512], in0=banks[bi][:],
                            in1=gab[d][:, hf * 512:(hf + 1) * 512], op=ALU.mult),
                           reads=[BK[bi], f"bgab{d}"], writes=["bzf"])
                    op('dve', lambda d=d: V.tensor_tensor(out=zb[:], in0=zf[:], in1=zpb[d][:], op=ALU.add),
                       reads=["bzf", f"bzpb{d}"], writes=["bzb"])
                    p1 = bank_bf(7)
                    for c in range(8):
                        op('pe', lambda c=c: T_.transpose(out=p1[:, c * 128:(c + 1) * 128],
                                                          in_=zb[:, c * 128:(c + 1) * 128], identity=identb[:]),
                           reads=["bzb", "identb"], writes=[BK[7]])
                    op('act', lambda: A_.copy(out=zT[:].rearrange("p a b -> p (a b)"), in_=p1),
                       reads=[BK[7]], writes=["bzT"])
                    for hf in range(2):
                        bi = (4, 2)[hf]
                        for c in range(8):
                            op('pe', lambda c=c, hf=hf, bi=bi: T_.matmul(
                                banks[bi][:], lhsT=zT[:, c, :], rhs=woutb[:, c, hf * 512:(hf + 1) * 512],
                                start=(c == 0), stop=(c == 7)),
                               reads=["bzT", "bwoutb"], writes=[BK[bi]])
                        op('dve', lambda hf=hf, bi=bi, d=d: V.tensor_tensor(
                            out=xt[d][:, hf * 512:(hf + 1) * 512], in0=banks[bi][:],
                            in1=xt[d][:, hf * 512:(hf + 1) * 512], op=ALU.add),
                           reads=[BK[bi], f"bxt{d}"], writes=[f"bxt{d}"])
                    dst = h_s if "peer" in phases else y_d
                    dma('pool', dst.ap()[t * 128:(t + 1) * 128, :], xt[d][:], reads=[f"bxt{d}"],
                        writes=["h_s"], slot=f"sh{d}")

                load_seq_ki(0)
                load_seq_kv(0)
                s1_load_diag(0)
                s1_index(0)
                s1_bisect(0)
                s2_load(0)
                if NTILE > 1:
                    if 1 % TPS == 0:
                        load_seq_ki(1 // TPS)
                    s1_load_diag(1)
                    s1_index(1)
                for t in range(NTILE):
                    b_, i = divmod(t, TPS)
                    if t + 2 < NTILE:
                        s1_load_diag(t + 2)
                    if t + 1 < NTILE:
                        s1_bisect(t + 1)
                    if t + 2 < NTILE:
                        if (t + 2) % TPS == 0:
                            load_seq_ki((t + 2) // TPS)
                        s1_index(t + 2)
                    stage2(t)
                    if t + 1 < NTILE:
                        if (t + 1) % TPS == 0:
                            load_seq_kv(b_ + 1)
                        s2_load(t + 1)
                S_.barrier()

        if "mix" in phases:
            mixer_a()
            mixer_b()
        else:
            with ExitStack() as st:
                tb = [sb(st, f"cp{i}", [128, D], F32) for i in range(2)]
                for t in range(NTILE):
                    b = t % 2
                    dma('sp', tb[b][:], x_d.ap()[t * 128:(t + 1) * 128, :], writes=[f"cp{b}"],
                        slot=f"cpi{b}")
                    dma('sp', h_s.ap()[t * 128:(t + 1) * 128, :], tb[b][:], reads=[f"cp{b}"],
                        writes=["h_s"], slot=f"cpo{b}")
                S_.barrier()
        if "peer" in phases:
            peer_phase()
        elif "copy" in phases:
            with ExitStack() as st:
                tb = [sb(st, f"cq{i}", [128, D], F32) for i in range(2)]
                for t in range(NTILE):
                    b = t % 2
                    dma('sp', tb[b][:], h_s.ap()[t * 128:(t + 1) * 128, :], reads=["h_s"], writes=[f"cq{b}"],
                        slot=f"cqi{b}")
                    dma('pool', y_d.ap()[t * 128:(t + 1) * 128, :], tb[b][:], reads=[f"cq{b}"],
                        writes=["y"], slot=f"cqo{b}")
                S_.barrier()
        S_.barrier()
    return nc


def host_consts(S):
    bf = ml_dtypes.bfloat16
    identb = np.eye(128, dtype=np.float32).astype(bf)
    iota128 = np.broadcast_to(np.arange(128, dtype=np.float32)[None, :], (128, 128)).astype(bf)
    c16 = np.zeros((128, 3, 16), np.float32)
    c16[:, 0, :] = 16.0 * np.arange(16)
    c16[:, 1, :] = np.arange(16)
    return dict(identb=identb, iota128=iota128, c16=c16)


def make_in_maps(inputs, n_cores, NB, S):
    f = np.float32
    x = np.asarray(inputs["x"], f)
    B = x.shape[0]
    assert B == n_cores * NB and x.shape[1] == S
    c = host_consts(S)

    def pk(v):
        return np.ascontiguousarray(np.asarray(v, f).reshape(8, 128).T)

    sk = np.asarray(inputs["peer_subkeys"], f)[0]
    skt = np.zeros((2, 64, 8, 2, 128), f)
    for p in range(2):
        skt[p, :, :, p, :] = sk[:, p].transpose(2, 0, 1)
    skt = np.ascontiguousarray(skt.reshape(128, 16, 128))
    half = 8
    inv_freq = (500000.0 ** (-np.arange(half, dtype=np.float32) / half)).astype(f)
    ang = np.arange(S, dtype=f)[:, None] * inv_freq[None, :]
    rope = np.stack([np.cos(ang), np.sin(ang)], axis=1).astype(f).reshape(S // 128, 128, 2, 8)
    gqk = np.stack([np.asarray(inputs["q_norm_g"], f)[0], np.asarray(inputs["k_norm_g"], f)[0]], 0)
    gqk = np.ascontiguousarray(np.broadcast_to(gqk[None], (128, 2, 64)))
    poolw = np.ascontiguousarray(np.asarray(inputs["pool_w"], f)[0].transpose(1, 0, 2))
    pscale = np.ascontiguousarray(np.asarray(inputs["pool_scale"], f)[0].reshape(4, 128).T)
    band = np.zeros((3, 128, 4, 128), f)
    for g, w in enumerate((2, 4, 8, 16)):
        for t in range(128):
            cnt0 = min(t + 1, w)
            for s_ in range(max(0, t - w + 1), t + 1):
                band[0, s_, g, t] += 1.0 / cnt0
                band[1, s_, g, t] += 1.0 / w
            band[0, t, g, t] -= 1.0
            band[1, t, g, t] -= 1.0
            for s_ in range(t - w + 1, 0):
                band[2, 128 + s_, g, t] += 1.0 / w
    bmask = np.zeros((128, 128), f)
    bmask[:64, 64:] = NEG
    shared = dict(
        g2=pk(inputs["norm2_g"][0]), wq=np.ascontiguousarray(np.asarray(inputs["peer_wq"], f)[0]),
        skt=skt, pu=np.ascontiguousarray(np.asarray(inputs["peer_u"], f)[0]),
        pv=np.ascontiguousarray(np.asarray(inputs["peer_v"], f)[0]),
        g1=pk(inputs["norm1_g"][0]), win=np.ascontiguousarray(np.asarray(inputs["w_in"], f)[0]),
        gqk=gqk, rope=rope, poolw=poolw, pscale=pscale,
        wba=np.ascontiguousarray(np.asarray(inputs["w_branch_attn"], f)[0]),
        wbp=np.ascontiguousarray(np.asarray(inputs["w_branch_pool"], f)[0]),
        wout=np.ascontiguousarray(np.asarray(inputs["w_out"], f)[0]),
        band=band, bmask=bmask, **c)
    maps = []
    for i in range(n_cores):
        m = dict(shared)
        m["x"] = np.ascontiguousarray(x[i * NB:(i + 1) * NB].reshape(NB * S, D))
        maps.append(m)
    return maps


_NC_CACHE = {}


def kernel(**inputs):
    n_cores = 8
    x = np.asarray(inputs["x"])
    B, S, _ = x.shape
    NB = B // n_cores
    key = (NB, S)
    if key not in _NC_CACHE:
        _NC_CACHE[key] = build(NB, S)
    nc = _NC_CACHE[key]
    maps = make_in_maps(inputs, n_cores, NB, S)
    res = run_bass_kernel_spmd(nc, maps, core_ids=list(range(n_cores)))
    out = np.stack([r["y"].reshape(NB, S, D) for r in res.results], 0).reshape(B, S, D)
    return out.astype(np.float32)
```

```python
import bisect
from contextlib import ExitStack

import numpy as np
import ml_dtypes
import concourse.bass as bass
import concourse.mybir as mybir
from concourse.bass_utils import run_bass_kernel_spmd

F32 = mybir.dt.float32
BF16 = mybir.dt.bfloat16
U32 = mybir.dt.uint32
AF = mybir.ActivationFunctionType
ALU = mybir.AluOpType
AX = mybir.AxisListType

D = 1024
EPS = 1e-6
NEG = -1.0e30


class Sched:
    def __init__(self, nc, es):
        self.nc = nc
        self.es = es
        self.E = dict(pe=nc.tensor, dve=nc.vector, act=nc.scalar, pool=nc.gpsimd, sp=nc.sync)
        self.sem = {k: es.enter_context(nc.semaphore("s_" + k)) for k in self.E}
        self.ins = {k: [] for k in self.E}
        self.marks = {k: [] for k in self.E}
        self.waited = {k: {} for k in self.E}
        self.res = {}
        self.dsem = {}

    def _resolve(self, dep):
        if dep[0] == 'd':
            return ('d', dep[1]), self.dsem[dep[1]][0], dep[2]
        _, e, idx = dep
        m = self.marks[e]
        if m and m[-1] >= idx:
            pos = bisect.bisect_left(m, idx)
            return ('c', e), self.sem[e], pos + 1
        self.ins[e][idx].then_inc(self.sem[e], 1)
        m.append(idx)
        return ('c', e), self.sem[e], len(m)

    def _wait(self, eng, deps):
        for dep in deps:
            if dep is None:
                continue
            if dep[0] == 'c' and dep[1] == 'pe' and eng == 'pe':
                continue
            key, sem, val = self._resolve(dep)
            if self.waited[eng].get(key, 0) >= val:
                continue
            self.E[eng].wait_ge(sem, val)
            self.waited[eng][key] = val

    def _deps(self, reads, writes):
        deps = []
        for r in reads:
            st = self.res.get(r)
            if st and st['w'] is not None:
                deps.append(st['w'])
        for w in writes:
            st = self.res.get(w)
            if st:
                if st['w'] is not None:
                    deps.append(st['w'])
                deps.extend(st['r'].values())
        return deps

    def _update(self, dep, rkey, reads, writes):
        for r in reads:
            st = self.res.setdefault(r, {'w': None, 'r': {}})
            st['r'][rkey] = dep
        for w in writes:
            self.res[w] = {'w': dep, 'r': {}}

    def op(self, eng, fn, reads=(), writes=()):
        self._wait(eng, self._deps(reads, writes))
        ins = fn()
        idx = len(self.ins[eng])
        self.ins[eng].append(ins)
        self._update(('c', eng, idx), eng, reads, writes)
        return ins

    def dma(self, q, out, in_, reads=(), writes=(), slot=None):
        deps = self._deps(reads, writes)
        if slot not in self.dsem:
            self.dsem[slot] = [self.es.enter_context(self.nc.semaphore("d_" + slot)), 0]
        s = self.dsem[slot]
        if s[1] > 0:
            deps.append(('d', slot, 16 * s[1]))
        self._wait(q, deps)
        s[1] += 1
        self.E[q].dma_start(out=out, in_=in_).then_inc(s[0], 16)
        self._update(('d', slot, 16 * s[1]), ('d', slot), reads, writes)

    def barrier(self):
        deps = []
        for e in self.E:
            if self.ins[e]:
                deps.append(('c', e, len(self.ins[e]) - 1))
        for slot, s in self.dsem.items():
            if s[1] > 0:
                deps.append(('d', slot, 16 * s[1]))
        for e in self.E:
            for dep in deps:
                key, sem, val = self._resolve(dep)
                if self.waited[e].get(key, 0) >= val:
                    continue
                self.E[e].wait_ge(sem, val)
                self.waited[e][key] = val
        self.res = {}


def AP(t, off, dims):
    return bass.AP(t, off, [list(d) for d in dims])


def build(NB, S, phases=("mix", "peer"), debug=False, stage=99):
    NT = NB * S
    NTILE = NT // 128
    TPS = S // 128
    nc = bass.Bass("TRN2", target_bir_lowering=False)

    def din(name, shape, dt=F32):
        return nc.dram_tensor(name, list(shape), dt, kind="ExternalInput")

    x_d = din("x", [NT, D])
    y_d = nc.dram_tensor("y", [NT, D], F32, kind="ExternalOutput")
    g2_d = din("g2", [128, 8])
    wq_d = din("wq", [D, D])
    skt_d = din("skt", [128, 16, 128])
    u_d = din("pu", [16384, D])
    v_d = din("pv", [16384, D])
    identb_d = din("identb", [128, 128], BF16)
    iota128_d = din("iota128", [128, 128], BF16)
    c16_d = din("c16", [128, 3, 16])
    g1_d = din("g1", [128, 8])
    win_d = din("win", [D, 4680])
    gqk_d = din("gqk", [128, 2, 64])
    rope_d = din("rope", [S // 128, 128, 2, 8])
    poolw_d = din("poolw", [128, 4, 128])
    pscale_d = din("pscale", [128, 4])
    wba_d = din("wba", [512, D])
    wbp_d = din("wbp", [512, D])
    wout_d = din("wout", [D, D])
    band_d = din("band", [3, 128, 4, 128])
    bmask_d = din("bmask", [128, 128])

    def dscr(name, shape, dt):
        return nc.dram_tensor(name, list(shape), dt, kind="Internal")

    ut_s = dscr("ut_s", [128, 128, D], BF16)
    v_s = dscr("v_s", [128, 128, D], BF16)
    h_s = dscr("h_s", [NT, D], F32)
    qT_s = dscr("qT_s", [NTILE, 128, 1024], BF16)
    kT_s = dscr("kT_s", [NB, 128, 4, S], BF16)
    v2_s = dscr("v2_s", [NB, S, 520], BF16)
    qiT_s = dscr("qiT_s", [NTILE, 128, 1024], BF16)
    kiT_s = dscr("kiT_s", [NB, 128, S], BF16)
    wi_s = dscr("wi_s", [NTILE, 128, 8], F32)
    ga_s = dscr("ga_s", [NTILE, 128, D], BF16)
    zp_s = dscr("zp_s", [NTILE, 128, D], BF16)
    dbg = {}

    es = ExitStack()
    with es:
        es.enter_context(nc.allow_low_precision(reason="bf16 matmul operands by design"))
        es.enter_context(nc.allow_non_contiguous_dma(reason="layout transforms"))
        S_ = Sched(nc, es)
        global LAST_SCHED
        LAST_SCHED = S_
        op = S_.op
        dma = S_.dma
        V, A_, P_, T_ = nc.vector, nc.scalar, nc.gpsimd, nc.tensor

        def sb(st, name, shape, dt):
            return st.enter_context(nc.sbuf_tensor("sb_" + name, list(shape), dt))

        def ps(st, name, shape, dt):
            return st.enter_context(nc.psum_tensor("ps_" + name, list(shape), dt))

        cst = ExitStack()
        es.enter_context(cst)
        identb = sb(cst, "identb", [128, 128], BF16)
        iota128 = sb(cst, "iota128", [128, 128], BF16)
        c16 = sb(cst, "c16", [128, 3, 16], F32)
        g2 = sb(cst, "g2", [128, 8], F32)
        g1 = sb(cst, "g1", [128, 8], F32)
        dma('sp', identb[:], identb_d.ap(), writes=["identb"], slot="c0")
        dma('sp', iota128[:], iota128_d.ap(), writes=["iota128"], slot="c1")
        dma('sp', c16[:], c16_d.ap(), writes=["c16"], slot="c2")
        dma('sp', g2[:], g2_d.ap(), writes=["g2"], slot="c3")
        dma('sp', g1[:], g1_d.ap(), writes=["g1"], slot="c4")

        banks = [ps(cst, f"bank{i}", [128, 512], F32) for i in range(8)]
        BK = [f"B{i}" for i in range(8)]

        def bank_bf(i):
            return banks[i][:].bitcast(BF16)

        def norm_transpose(st_tag, src, src_key, junk, ssq, rstd, xnb, g_t, g_key,
                           xnT_ap_fn, xnT_key, bank_i, op_=None):
            op = op_ or S_.op
            op('act', lambda: A_.activation(out=junk[:], in_=src[:], func=AF.Square,
                                            accum_out=ssq[:]),
               reads=[src_key], writes=[st_tag + "junk", st_tag + "ssq"])
            op('dve', lambda: V.tensor_scalar(out=rstd[:], in0=ssq[:], scalar1=1.0 / D,
                                              scalar2=EPS, op0=ALU.mult, op1=ALU.add),
               reads=[st_tag + "ssq"], writes=[st_tag + "rstd"])
            op('act', lambda: A_.activation(out=rstd[:], in_=rstd[:], func=AF.Sqrt),
               reads=[st_tag + "rstd"], writes=[st_tag + "rstd"])
            op('dve', lambda: V.reciprocal(out=rstd[:], in_=rstd[:]),
               reads=[st_tag + "rstd"], writes=[st_tag + "rstd"])
            op('dve', lambda: V.tensor_scalar(out=xnb[:], in0=src[:], scalar1=rstd[:, 0:1],
                                              scalar2=None, op0=ALU.mult),
               reads=[src_key, st_tag + "rstd"], writes=[st_tag + "xnb"])
            pb = bank_bf(bank_i)
            for kc in range(8):
                op('pe', lambda kc=kc: T_.transpose(out=pb[:, kc * 128:(kc + 1) * 128],
                                                    in_=xnb[:, kc * 128:(kc + 1) * 128],
                                                    identity=identb[:]),
                   reads=[st_tag + "xnb", "identb"], writes=[BK[bank_i]])
            pbv = AP(pb.tensor, pb.offset, [pb.ap[0], [128, 8], [1, 128]])
            gv = AP(g_t, 0, [[8, 128], [1, 8], [0, 128]])
            op('dve', lambda: V.tensor_tensor(out=xnT_ap_fn(), in0=pbv, in1=gv, op=ALU.mult),
               reads=[BK[bank_i], g_key], writes=[xnT_key])


        prep_done = [False]

        class Deferred:
            def __init__(self):
                self.q = []

            def op(self, *a, **k):
                self.q.append((op, a, k))

            def dma(self, *a, **k):
                self.q.append((dma, a, k))

            def run(self, n):
                while n > 0 and self.q:
                    f, a, k = self.q.pop(0)
                    f(*a, **k)
                    n -= 1

        def prep_ops(st, op, dma):
            prep_done[0] = True
            ub = [sb(st, f"ub{i}", [128, D], F32) for i in range(2)]
            ubb = [sb(st, f"ubb{i}", [128, D], BF16) for i in range(2)]
            utb = [sb(st, f"utb{i}", [128, D], BF16) for i in range(2)]
            vb = [sb(st, f"vb{i}", [128, D], F32) for i in range(2)]
            vbb = [sb(st, f"vbb{i}", [128, D], BF16) for i in range(2)]
            for c in range(128):
                b = c % 2
                dma('sp', ub[b][:], u_d.ap()[c * 128:(c + 1) * 128, :],
                    writes=[f"ub{b}"], slot=f"ub{b}")
                dma('sp', vb[b][:], v_d.ap()[c * 128:(c + 1) * 128, :],
                    writes=[f"vb{b}"], slot=f"vb{b}")
                op('dve', lambda b=b: V.tensor_copy(out=ubb[b][:], in_=ub[b][:]),
                   reads=[f"ub{b}"], writes=[f"ubb{b}"])
                op('pool', lambda b=b: P_.tensor_copy(out=vbb[b][:], in_=vb[b][:]),
                   reads=[f"vb{b}"], writes=[f"vbb{b}"])
                bi = 6 + b
                pb = bank_bf(bi)
                for kc in range(8):
                    op('pe', lambda kc=kc, b=b, pb=pb: T_.transpose(
                        out=pb[:, kc * 128:(kc + 1) * 128],
                        in_=ubb[b][:, kc * 128:(kc + 1) * 128], identity=identb[:]),
                       reads=[f"ubb{b}", "identb"], writes=[BK[bi]])
                op('act', lambda b=b, pb=pb: A_.copy(out=utb[b][:], in_=pb),
                   reads=[BK[bi]], writes=[f"utb{b}"])
                dma('pool', ut_s.ap()[c], utb[b][:], reads=[f"utb{b}"], writes=["ut_s"],
                    slot=f"uts{b}")
                dma('pool', v_s.ap()[c], vbb[b][:], reads=[f"vbb{b}"], writes=["v_s"],
                    slot=f"vs{b}")

        def peer_phase():
            T = 256
            NBLK = NT // T
            if not prep_done[0]:
                with ExitStack() as st:
                    prep_ops(st, op, dma)
                    S_.barrier()
            if stage <= 1:
                return

            with ExitStack() as st:
                wqb = sb(st, "wqb", [128, 8, D], BF16)
                sktb = sb(st, "sktb", [128, 16, 128], BF16)
                with ExitStack() as st2:
                    wtmp = sb(st2, "wtmp", [128, 8, D], F32)
                    stmp = sb(st2, "stmp", [128, 16, 128], F32)
                    dma('sp', wtmp[:], wq_d.ap().rearrange("(k p) c -> p k c", p=128),
                        writes=["wtmp"], slot="c5")
                    dma('sp', stmp[:], skt_d.ap(), writes=["stmp"], slot="c6")
                    op('dve', lambda: V.tensor_copy(out=wqb[:], in_=wtmp[:]),
                       reads=["wtmp"], writes=["wqb"])
                    op('dve', lambda: V.tensor_copy(out=sktb[:], in_=stmp[:]),
                       reads=["stmp"], writes=["sktb"])
                    S_.barrier()

                WT2 = sb(st, "WT2", [128, T, 128], BF16)
                NSB = 5
                utc = [sb(st, f"utc{i}", [128, D], BF16) for i in range(NSB)]
                vc = [sb(st, f"vc{i}", [128, D], BF16) for i in range(NSB)]
                xnTs = [sb(st, f"xnT{i}", [128, 8, T], BF16) for i in range(2)]
                hbs = [[sb(st, f"hb{i}{j}", [128, D], F32) for j in range(2)] for i in range(2)]
                junk = sb(st, "pjunk", [128, D], BF16)
                xnb = sb(st, "pxnb", [128, D], BF16)
                ssq = sb(st, "pssq", [128, 1], F32)
                rstd = sb(st, "prstd", [128, 1], F32)
                qT = sb(st, "pqT", [128, 8, T], BF16)
                sub = sb(st, "psub", [128, 16, 128], F32)
                sub2 = sb(st, "psub2", [128, 16, 128], F32)
                a16 = sb(st, "pa16", [128, 16, 16], F32)
                ixu = sb(st, "pixu", [128, 16, 16], U32)
                ixf = sb(st, "pixf", [128, 16, 16], F32)
                cand = sb(st, "pcand", [128, 8, 256], F32)
                cand2 = sb(st, "pcand2", [128, 8, 256], F32)
                ts = sb(st, "pts", [128, 8, 16], F32)
                posu = sb(st, "pposu", [128, 8, 16], U32)
                posf = sb(st, "pposf", [128, 128], F32)
                e0 = sb(st, "pe0", [128, 128, 16], F32)
                e1 = sb(st, "pe1", [128, 128, 16], F32)
                r0f = sb(st, "pr0f", [128, 128], F32)
                r1f = sb(st, "pr1f", [128, 128], F32)
                zs = sb(st, "pzs", [128, 8], F32)
                trin = sb(st, "ptrin", [128, 3, 128], BF16)
                trTs = [[sb(st, f"ptrT{i}{j}", [128, 3, 128], BF16) for j in range(2)] for i in range(2)]
                Lc = [sb(st, f"pLc{i}", [128, 8, 128], BF16) for i in range(2)]
                Rc = [sb(st, f"pRc{i}", [128, 8, 128], BF16) for i in range(2)]
                gact = [sb(st, f"pga{i}", [128, T], BF16) for i in range(3)]
                gw = [sb(st, f"pgw{i}", [128, T], BF16) for i in range(3)]

                def r1(blk, op, dma):
                    par = blk % 2
                    xnT = xnTs[par]
                    hb = hbs[par]
                    for tt in range(2):
                        tile = blk * 2 + tt
                        tk = f"hb{par}{tt}"
                        dma('sp', hb[tt][:], h_s.ap()[tile * 128:(tile + 1) * 128, :],
                            reads=["h_s"], writes=[tk], slot=tk)
                        norm_transpose("p", hb[tt], tk, junk, ssq, rstd, xnb, g2, "g2",
                                       lambda tt=tt: xnT[:, :, tt * 128:(tt + 1) * 128],
                                       f"xnT{par}", 7, op_=op)
                    for c in range(8):
                        bi = 7
                        for kc in range(8):
                            op('pe', lambda c=c, kc=kc, bi=bi: T_.matmul(
                                banks[bi][:, 0:T], lhsT=wqb[:, kc, c * 128:(c + 1) * 128],
                                rhs=xnT[:, kc, :], start=(kc == 0), stop=(kc == 7)),
                               reads=["wqb", f"xnT{par}"], writes=[BK[bi]])
                        op('act', lambda c=c, bi=bi: A_.copy(out=qT[:, c, :], in_=banks[bi][:, 0:T]),
                           reads=[BK[bi]], writes=["pqT"])
                    def tile_body(tt):
                        trT = trTs[par][tt]
                        for r in range(4):
                            bi = 7
                            for gg in range(4):
                                g = r * 4 + gg
                                h = g // 2
                                op('pe', lambda h=h, g=g, gg=gg, bi=bi, tt=tt: T_.matmul(
                                    banks[bi][:, gg * 128:(gg + 1) * 128],
                                    lhsT=qT[:, h, tt * 128:(tt + 1) * 128],
                                    rhs=sktb[:, g, :],
                                    start=True, stop=True),
                                   reads=["pqT", "sktb"], writes=[BK[bi]])
                            op('act', lambda bi=bi, r=r: A_.copy(
                                out=sub[:, r * 4:(r + 1) * 4, :], in_=banks[bi][:]),
                               reads=[BK[bi]], writes=["psub"])
                        for g in range(16):
                            op('dve', lambda g=g: V.max(out=a16[:, g, 0:8], in_=sub[:, g, :]),
                               reads=["psub"], writes=[f"pa16_{g}"])
                        for g in range(16):
                            op('dve', lambda g=g: V.max_index(out=ixu[:, g, 0:8], in_max=a16[:, g, 0:8],
                                                             in_values=sub[:, g, :]),
                               reads=["psub", f"pa16_{g}"], writes=[f"pixu_{g}"])
                        for g in range(16):
                            op('dve', lambda g=g: V.match_replace(out=sub2[:, g, :],
                                                                 in_to_replace=a16[:, g, 0:8],
                                                                 in_values=sub[:, g, :], imm_value=NEG),
                               reads=["psub", f"pa16_{g}"], writes=[f"psub2_{g}"])
                        for g in range(16):
                            op('dve', lambda g=g: V.max(out=a16[:, g, 8:16], in_=sub2[:, g, :]),
                               reads=[f"psub2_{g}"], writes=[f"pa16_{g}"])
                        for g in range(16):
                            op('dve', lambda g=g: V.max_index(out=ixu[:, g, 8:16], in_max=a16[:, g, 8:16],
                                                             in_values=sub2[:, g, :]),
                               reads=[f"psub2_{g}", f"pa16_{g}"], writes=[f"pixu_{g}"])
                        op('dve', lambda: V.tensor_copy(out=ixf[:], in_=ixu[:]),
                           reads=[f"pixu_{g}" for g in range(16)], writes=["pixf"])
                        in0 = AP(a16, 0, [[256, 128], [32, 8], [1, 16], [0, 16]])
                        in1 = AP(a16, 16, [[256, 128], [32, 8], [0, 16], [1, 16]])
                        candv = AP(cand, 0, [[2048, 128], [256, 8], [16, 16], [1, 16]])
                        op('dve', lambda: V.tensor_tensor(out=candv, in0=in0, in1=in1, op=ALU.add),
                           reads=[f"pa16_{g}" for g in range(16)], writes=["pcand"])
                        for h in range(8):
                            op('dve', lambda h=h: V.max(out=ts[:, h, 0:8], in_=cand[:, h, :]),
                               reads=["pcand"], writes=[f"pts_{h}"])
                        for h in range(8):
                            op('dve', lambda h=h: V.max_index(out=posu[:, h, 0:8], in_max=ts[:, h, 0:8],
                                                             in_values=cand[:, h, :]),
                               reads=["pcand", f"pts_{h}"], writes=[f"pposu_{h}"])
                        for h in range(8):
                            op('dve', lambda h=h: V.match_replace(out=cand2[:, h, :],
                                                                 in_to_replace=ts[:, h, 0:8],
                                                                 in_values=cand[:, h, :], imm_value=NEG),
                               reads=["pcand", f"pts_{h}"], writes=[f"pcand2_{h}"])
                        for h in range(8):
                            op('dve', lambda h=h: V.max(out=ts[:, h, 8:16], in_=cand2[:, h, :]),
                               reads=[f"pcand2_{h}"], writes=[f"pts_{h}"])
                        for h in range(8):
                            op('dve', lambda h=h: V.max_index(out=posu[:, h, 8:16], in_max=ts[:, h, 8:16],
                                                             in_values=cand2[:, h, :]),
                               reads=[f"pcand2_{h}", f"pts_{h}"], writes=[f"pposu_{h}"])
                        op('dve', lambda: V.tensor_copy(out=posf[:], in_=posu[:].rearrange("p a b -> p (a b)")),
                           reads=[f"pposu_{h}" for h in range(8)], writes=["pposf"])
                        posb = AP(posf, 0, [[128, 128], [1, 128], [0, 16]])
                        c16b = AP(c16, 0, [[48, 128], [0, 128], [1, 16]])
                        iob = AP(c16, 16, [[48, 128], [0, 128], [1, 16]])
                        op('dve', lambda: V.tensor_tensor(out=e0[:], in0=posb, in1=c16b, op=ALU.subtract),
                           reads=["pposf", "c16"], writes=["pe0"])
                        op('dve', lambda: V.tensor_scalar(out=e1[:], in0=e0[:], scalar1=0.0, scalar2=None,
                                                          op0=ALU.is_ge),
                           reads=["pe0"], writes=["pe1"])
                        op('dve', lambda: V.tensor_scalar(out=e0[:], in0=e0[:], scalar1=16.0, scalar2=None,
                                                          op0=ALU.is_lt),
                           reads=["pe0"], writes=["pe0"])
                        op('dve', lambda: V.tensor_tensor(out=e0[:], in0=e0[:], in1=e1[:], op=ALU.mult),
                           reads=["pe0", "pe1"], writes=["pe0"])
                        op('dve', lambda: V.tensor_tensor(out=e1[:], in0=e0[:], in1=iob, op=ALU.mult),
                           reads=["pe0", "c16"], writes=["pe1"])
                        op('dve', lambda: V.tensor_reduce(out=r0f[:], in_=e1[:], axis=AX.X, op=ALU.add),
                           reads=["pe1"], writes=["pr0f"])
                        ix0b = AP(ixf, 0, [[256, 128], [32, 8], [0, 16], [1, 16]])
                        ix1b = AP(ixf, 16, [[256, 128], [32, 8], [0, 16], [1, 16]])
                        e0v = AP(e0, 0, [[2048, 128], [256, 8], [16, 16], [1, 16]])
                        e1v = AP(e1, 0, [[2048, 128], [256, 8], [16, 16], [1, 16]])
                        op('dve', lambda: V.tensor_tensor(out=e0v, in0=e0v, in1=ix0b, op=ALU.mult),
                           reads=["pe0", "pixf"], writes=["pe0"])
                        op('dve', lambda: V.tensor_reduce(out=trin[:, 0, :], in_=e0[:], axis=AX.X, op=ALU.add),
                           reads=["pe0"], writes=["ptrin"])
                        op('dve', lambda: V.scalar_tensor_tensor(out=r1f[:], in0=r0f[:], scalar=-16.0,
                                                                 in1=posf[:], op0=ALU.mult, op1=ALU.add),
                           reads=["pr0f", "pposf"], writes=["pr1f"])
                        r1b = AP(r1f, 0, [[128, 128], [1, 128], [0, 16]])
                        op('dve', lambda: V.tensor_tensor(out=e1[:], in0=iob, in1=r1b, op=ALU.is_equal),
                           reads=["pr1f", "c16"], writes=["pe1"])
                        op('dve', lambda: V.tensor_tensor(out=e1v, in0=e1v, in1=ix1b, op=ALU.mult),
                           reads=["pe1", "pixf"], writes=["pe1"])
                        op('dve', lambda: V.tensor_reduce(out=trin[:, 1, :], in_=e1[:], axis=AX.X, op=ALU.add),
                           reads=["pe1"], writes=["ptrin"])
                        op('dve', lambda: V.tensor_copy(out=zs[:], in_=ts[:, :, 0]),
                           reads=[f"pts_{h}" for h in range(8)], writes=["pzs"])
                        mxb = AP(zs, 0, [[8, 128], [1, 8], [0, 16]])
                        op('dve', lambda: V.tensor_tensor(out=ts[:], in0=ts[:], in1=mxb, op=ALU.subtract),
                           reads=[f"pts_{h}" for h in range(8)] + ["pzs"], writes=[f"pts_{h}" for h in range(8)])
                        op('act', lambda: A_.activation(out=ts[:], in_=ts[:], func=AF.Exp),
                           reads=[f"pts_{h}" for h in range(8)], writes=[f"pts_{h}" for h in range(8)])
                        op('dve', lambda: V.tensor_reduce(out=zs[:], in_=ts[:], axis=AX.X, op=ALU.add),
                           reads=[f"pts_{h}" for h in range(8)], writes=["pzs"])
                        op('dve', lambda: V.reciprocal(out=zs[:], in_=zs[:]),
                           reads=["pzs"], writes=["pzs"])
                        zb = AP(zs, 0, [[8, 128], [1, 8], [0, 16]])
                        gout = AP(trin, 256, [[384, 128], [16, 8], [1, 16]])
                        op('dve', lambda: V.tensor_tensor(out=gout, in0=ts[:], in1=zb, op=ALU.mult),
                           reads=[f"pts_{h}" for h in range(8)] + ["pzs"], writes=["ptrin"])
                        pb = bank_bf(7)
                        for w in range(3):
                            op('pe', lambda w=w, pb=pb: T_.transpose(out=pb[:, w * 128:(w + 1) * 128],
                                                                     in_=trin[:, w, :], identity=identb[:]),
                               reads=["ptrin", "identb"], writes=[BK[7]])
                        op('act', lambda pb=pb: A_.copy(out=trT[:].rearrange("p a b -> p (a b)"),
                                                        in_=pb[:, 0:384]),
                           reads=[BK[7]], writes=[f"ptrT{par}{tt}"])

                    for tt in range(2):
                        tile_body(tt)

                def r2(blk):
                    par = blk % 2
                    for tt in range(2):
                        trT = trTs[par][tt]
                        for q8 in range(16):
                            lb = q8 % 2
                            t0 = q8 * 8
                            for tl in range(8):
                                t = t0 + tl
                                op('dve', lambda lb=lb, tl=tl, t=t: V.tensor_scalar(
                                    out=Lc[lb][:, tl, :], in0=iota128[:], scalar1=trT[:, 0, t:t + 1], scalar2=None,
                                    op0=ALU.is_equal),
                                   reads=["iota128", f"ptrT{par}{tt}"], writes=[f"pLc{lb}_{tl}"])
                                op('dve', lambda lb=lb, tl=tl, t=t: V.tensor_scalar(
                                    out=Rc[lb][:, tl, :], in0=iota128[:], scalar1=trT[:, 1, t:t + 1],
                                    scalar2=trT[:, 2, t:t + 1], op0=ALU.is_equal, op1=ALU.mult),
                                   reads=["iota128", f"ptrT{par}{tt}"], writes=[f"pRc{lb}_{tl}"])
                            for q in range(2):
                                bi = 4 + ((q8 * 2 + q) % 2)
                                for t4 in range(4):
                                    tl = q * 4 + t4
                                    op('pe', lambda lb=lb, tl=tl, t4=t4, bi=bi: T_.matmul(
                                        banks[bi][:, t4 * 128:(t4 + 1) * 128],
                                        lhsT=Rc[lb][:, tl, :], rhs=Lc[lb][:, tl, :],
                                        start=True, stop=True),
                                       reads=[f"pRc{lb}_{tl}", f"pLc{lb}_{tl}"], writes=[BK[bi]])
                                tg = tt * 128 + t0 + q * 4
                                op('act', lambda bi=bi, tg=tg: A_.copy(
                                    out=WT2[:, tg:tg + 4, :].rearrange("p a b -> p (a b)"),
                                    in_=banks[bi][:]),
                                   reads=[BK[bi]], writes=["WT2"])

                def chunks(blk, filler):
                    par = blk % 2
                    xnT = xnTs[par]
                    def ld(c):
                        b = c % NSB
                        dma('sp', utc[b][:], ut_s.ap()[c], reads=["ut_s"], writes=[f"utc{b}"], slot=f"utc{b}")
                        dma('sp', vc[b][:], v_s.ap()[c], reads=["v_s"], writes=[f"vc{b}"], slot=f"vc{b}")

                    def emitA(c):
                        b = c % NSB
                        bi = 4 + c % 3
                        for kc in range(8):
                            op('pe', lambda kc=kc, b=b, bi=bi: T_.matmul(
                                banks[bi][:, 0:T], lhsT=utc[b][:, kc * 128:(kc + 1) * 128],
                                rhs=xnT[:, kc, :], start=(kc == 0), stop=(kc == 7)),
                               reads=[f"utc{b}", f"xnT{par}"], writes=[BK[bi]])

                    def emitB(c):
                        b = c % NSB
                        g2_ = c % 3
                        bi = 4 + c % 3
                        op('act', lambda g2_=g2_, bi=bi: A_.activation(out=gact[g2_][:], in_=banks[bi][:, 0:T],
                                                                     func=AF.Gelu),
                           reads=[BK[bi]], writes=[f"pga{g2_}"])
                        wv = AP(WT2, c, [[T * 128, 128], [128, T]])
                        op('pool', lambda g2_=g2_, wv=wv: P_.tensor_tensor(out=gw[g2_][:], in0=gact[g2_][:], in1=wv,
                                                                       op=ALU.mult),
                           reads=[f"pga{g2_}", "WT2"], writes=[f"pgw{g2_}"])
                        for tt in range(2):
                            for hf in range(2):
                                bo = tt * 2 + hf
                                op('pe', lambda b=b, g2_=g2_, tt=tt, hf=hf, bo=bo, c=c: T_.matmul(
                                    banks[bo][:], lhsT=gw[g2_][:, tt * 128:(tt + 1) * 128],
                                    rhs=vc[b][:, hf * 512:(hf + 1) * 512],
                                    start=(c == 0), stop=(c == 127)),
                                   reads=[f"pgw{g2_}", f"vc{b}"], writes=[BK[bo]])

                    for c in range(min(NSB - 1, 128)):
                        ld(c)
                    emitA(0)
                    emitA(1)
                    for c in range(128):
                        filler()
                        if c + NSB - 1 < 128:
                            ld(c + NSB - 1)
                        if c + 2 < 128:
                            emitA(c + 2)
                        emitB(c)

                def epi(blk):
                    par = blk % 2
                    hb = hbs[par]
                    for tt in range(2):
                        tile = blk * 2 + tt
                        for hf in range(2):
                            bo = tt * 2 + hf
                            op('dve', lambda tt=tt, hf=hf, bo=bo: V.tensor_tensor(
                                out=hb[tt][:, hf * 512:(hf + 1) * 512], in0=banks[bo][:],
                                in1=hb[tt][:, hf * 512:(hf + 1) * 512], op=ALU.add),
                               reads=[BK[bo], f"hb{par}{tt}"], writes=[f"hb{par}{tt}"])
                        dma('pool', y_d.ap()[tile * 128:(tile + 1) * 128, :], hb[tt][:],
                            reads=[f"hb{par}{tt}"], writes=["y"], slot=f"y{tt}")


                r1(0, op, dma)
                r2(0)
                for blk in range(NBLK):
                    dq = Deferred()
                    if blk + 1 < NBLK:
                        r1(blk + 1, dq.op, dq.dma)
                    per = (len(dq.q) + 126) // 127
                    chunks(blk, lambda: dq.run(per))
                    dq.run(10 ** 9)
                    if blk + 1 < NBLK:
                        r2(blk + 1)
                    epi(blk)
                S_.barrier()


        def mixer_a():
            CH = [(0, 512), (512, 1024), (1024, 1536), (1536, 2048), (2048, 2120), (2120, 2632),
                  (2632, 3144), (3144, 3656), (3656, 4168), (4168, 4680)]
            with ExitStack() as st:
                winb = sb(st, "winb", [128, 8, 4680], BF16)
                wbpb = sb(st, "wbpb", [128, 4, D], BF16)
                poolwb = sb(st, "poolwb", [128, 4, 128], BF16)
                bandt = sb(st, "bandt", [128, 3, 4, 128], F32)
                pscale = sb(st, "pscale", [128, 4], F32)
                gqk = sb(st, "gqk", [128, 2, 64], F32)
                ropeall = sb(st, "ropeall", [128, TPS, 16], F32)
                with ExitStack() as st2:
                    wst = [sb(st2, f"wst{i}", [128, 4680], F32) for i in range(2)]
                    for kc in range(8):
                        b = kc % 2
                        dma('sp', wst[b][:], win_d.ap()[kc * 128:(kc + 1) * 128, :], writes=[f"wst{b}"],
                            slot=f"wst{b}")
                        op('dve' if b == 0 else 'pool',
                           (lambda kc=kc, b=b: V.tensor_copy(out=winb[:, kc, :], in_=wst[b][:])) if b == 0 else
                           (lambda kc=kc, b=b: P_.tensor_copy(out=winb[:, kc, :], in_=wst[b][:])),
                           reads=[f"wst{b}"], writes=["winb"])
                    dma('sp', wst[0][:, 0:4096].rearrange("p (a b) -> p a b", a=4),
                        wbp_d.ap().rearrange("(a p) c -> p a c", p=128), writes=["wst0"], slot="wst0")
                    op('dve', lambda: V.tensor_copy(out=wbpb[:].rearrange("p a b -> p (a b)"), in_=wst[0][:, 0:4096]),
                       reads=["wst0"], writes=["wbpb"])
                    dma('sp', wst[1][:, 0:512].rearrange("p (a b) -> p a b", a=4), poolw_d.ap(),
                        writes=["wst1"], slot="wst1")
                    op('dve', lambda: V.tensor_copy(out=poolwb[:].rearrange("p a b -> p (a b)"), in_=wst[1][:, 0:512]),
                       reads=["wst1"], writes=["poolwb"])
                    dma('sp', bandt[:], band_d.ap().rearrange("k s g t -> s k g t"), writes=["bandt"], slot="c5")
                    dma('sp', pscale[:], pscale_d.ap(), writes=["pscale"], slot="c6")
                    dma('sp', gqk[:], gqk_d.ap(), writes=["gqk"], slot="c7")
                    dma('sp', ropeall[:].rearrange("p i (a b) -> p i a b", a=2),
                        rope_d.ap().rearrange("i p a b -> p i a b"), writes=["ropeall"], slot="c8")
                    S_.barrier()

                xt = [sb(st, f"xt{i}", [128, D], F32) for i in range(2)]
                junk = sb(st, "ajunk", [128, D], BF16)
                xnb = sb(st, "axnb", [128, D], BF16)
                ssq = sb(st, "assq", [128, 1], F32)
                rstd = sb(st, "arstd", [128, 1], F32)
                xnT = sb(st, "axnT", [128, 8, 128], BF16)
                pj = sb(st, "apj", [128, 2176], F32)
                sq = sb(st, "asq", [128, 512], F32)
                s8 = sb(st, "as8", [128, 8], F32)
                pp = [sb(st, f"app{i}", [128, 512], F32) for i in range(2)]
                qkb = sb(st, "aqkb", [128, 2176], BF16)
                vaug = sb(st, "avaug", [128, 8, 65], BF16)
                qTz = sb(st, "aqTz", [128, 4, 2, 128], BF16)
                qiTz = sb(st, "aqiTz", [128, 4, 2, 128], BF16)
                kT = sb(st, "akT", [128, 4, 128], BF16)
                kiT = sb(st, "akiT", [128, 128], BF16)
                wis = sb(st, "awis", [128, 8], F32)
                gab = sb(st, "agab", [128, D], BF16)
                gpb = sb(st, "agpb", [128, D], BF16)
                zpb = sb(st, "azpb", [128, D], BF16)
                pooledT = sb(st, "apooledT", [128, 4, 128], BF16)
                mixedT = sb(st, "amixedT", [128, 4, 128], BF16)
                rt = sb(st, "art", [128, 4, 34, 8], F32)
                op('dve', lambda: V.memset(vaug[:], 1.0), writes=["avaug"])
                op('dve', lambda: V.memset(qTz[:], 0.0), writes=["aqTz"])
                op('dve', lambda: V.memset(qiTz[:], 0.0), writes=["aqiTz"])
                op('dve', lambda: V.memset(pj[:], 0.0), writes=["apj"])

                pq = Deferred()
                if "peer" in phases:
                    prep_ops(st, pq.op, pq.dma)
                pper = (len(pq.q) + NTILE - 1) // NTILE
                for t in range(NTILE):
                    pq.run(pper)
                    b_, i = divmod(t, TPS)
                    xb = t % 2
                    dma('sp', xt[xb][:], x_d.ap()[t * 128:(t + 1) * 128, :], writes=[f"xt{xb}"], slot=f"xt{xb}")
                    norm_transpose("a", xt[xb], f"xt{xb}", junk, ssq, rstd, xnb, g1, "g1",
                                   lambda: xnT[:], "axnT", 7)
                    pc = pp[i % 2]
                    pck = f"app{i % 2}"
                    for ci, (c0, c1) in enumerate(CH):
                        n = c1 - c0
                        bi = ci % 4
                        for kc in range(8):
                            op('pe', lambda kc=kc, bi=bi, c0=c0, c1=c1, n=n: T_.matmul(
                                banks[bi][:, 0:n], lhsT=xnT[:, kc, :], rhs=winb[:, kc, c0:c1],
                                start=(kc == 0), stop=(kc == 7)),
                               reads=["axnT", "winb"], writes=[BK[bi]])
                        if ci in (0, 1):
                            op('act', lambda bi=bi: A_.activation(out=sq[:], in_=banks[bi][:], func=AF.Square),
                               reads=[BK[bi]], writes=["asq"])
                            op('dve', lambda: V.tensor_reduce(out=s8[:], in_=sq[:].rearrange("p (a b) -> p a b", a=8),
                                                              axis=AX.X, op=ALU.add),
                               reads=["asq"], writes=["as8"])
                            op('dve', lambda: V.tensor_scalar(out=s8[:], in0=s8[:], scalar1=1.0 / 64, scalar2=EPS,
                                                              op0=ALU.mult, op1=ALU.add),
                               reads=["as8"], writes=["as8"])
                            op('act', lambda: A_.activation(out=s8[:], in_=s8[:], func=AF.Sqrt),
                               reads=["as8"], writes=["as8"])
                            op('dve', lambda: V.reciprocal(out=s8[:], in_=s8[:]), reads=["as8"], writes=["as8"])
                            pjv = AP(pj, c0, [[2176, 128], [64, 8], [1, 64]])
                            psv = AP(banks[bi], 0, [[512, 128], [64, 8], [1, 64]])
                            s8b = AP(s8, 0, [[8, 128], [1, 8], [0, 64]])
                            gb = AP(gqk, ci * 64, [[128, 128], [0, 8], [1, 64]])
                            op('dve', lambda pjv=pjv, psv=psv, s8b=s8b: V.tensor_tensor(out=pjv, in0=psv, in1=s8b,
                                                                                    op=ALU.mult),
                               reads=[BK[bi], "as8"], writes=["apj"])
                            op('dve', lambda pjv=pjv, gb=gb: V.tensor_tensor(out=pjv, in0=pjv, in1=gb, op=ALU.mult),
                               reads=["apj", "gqk"], writes=["apj"])
                        elif ci == 2:
                            psv = AP(banks[bi], 0, [[512, 128], [64, 8], [1, 64]])
                            op('act', lambda psv=psv: A_.copy(out=vaug[:, :, 0:64], in_=psv),
                               reads=[BK[bi]], writes=["avaug"])
                            dma('pool', v2_s.ap()[b_, i * 128:(i + 1) * 128, :],
                                vaug[:].rearrange("p a b -> p (a b)"), reads=["avaug"], writes=["v2_s"], slot="sv")
                        elif ci in (3, 4):
                            op('act', lambda bi=bi, c0=c0, c1=c1, n=n: A_.copy(out=pj[:, c0:c1], in_=banks[bi][:, 0:n]),
                               reads=[BK[bi]], writes=["apj"])
                        elif ci == 5:
                            op('act', lambda bi=bi, pc=pc: A_.copy(out=pc[:], in_=banks[bi][:]),
                               reads=[BK[bi]], writes=[pck])
                        else:
                            dst = gab if ci in (6, 7) else gpb
                            dk = "agab" if ci in (6, 7) else "agpb"
                            hf = ci % 2
                            op('act', lambda bi=bi, dst=dst, hf=hf: A_.activation(
                                out=dst[:, hf * 512:(hf + 1) * 512], in_=banks[bi][:], func=AF.Sigmoid),
                               reads=[BK[bi]], writes=[dk])
                    x1 = AP(pj, 0, [[2176, 128], [64, 33], [1, 8]])
                    x2 = AP(pj, 8, [[2176, 128], [64, 33], [1, 8]])
                    cosb = AP(ropeall, i * 16, [[TPS * 16, 128], [0, 33], [1, 8]])
                    sinb = AP(ropeall, i * 16 + 8, [[TPS * 16, 128], [0, 33], [1, 8]])
                    rts = [AP(rt, k * 272, [[1088, 128], [8, 33], [1, 8]]) for k in range(4)]
                    for k, (a_, b2) in enumerate(((x1, cosb), (x2, sinb), (x2, cosb), (x1, sinb))):
                        op('dve', lambda k=k, a_=a_, b2=b2: V.tensor_tensor(out=rts[k], in0=a_, in1=b2, op=ALU.mult),
                           reads=["apj", "ropeall"], writes=[f"art{k}"])
                    op('dve', lambda: V.tensor_tensor(out=x1, in0=rts[0], in1=rts[1], op=ALU.subtract),
                       reads=["art0", "art1"], writes=["apj"])
                    op('dve', lambda: V.tensor_tensor(out=x2, in0=rts[2], in1=rts[3], op=ALU.add),
                       reads=["art2", "art3"], writes=["apj"])
                    op('act', lambda: A_.copy(out=qkb[:, 0:1024], in_=pj[:, 0:1024]), reads=["apj"], writes=["aqkb"])
                    op('act', lambda: A_.copy(out=qkb[:, 1536:2112], in_=pj[:, 1536:2112]), reads=["apj"],
                       writes=["aqkb"])
                    op('act', lambda: A_.copy(out=qkb[:, 2112:2176], in_=pj[:, 2048:2112]), reads=["apj"],
                       writes=["aqkb"])
                    op('dve', lambda: V.tensor_scalar(out=wis[:], in0=pj[:, 2112:2120], scalar1=(8 ** -0.5) * 0.125,
                                                      scalar2=None, op0=ALU.mult),
                       reads=["apj"], writes=["awis"])
                    dma('pool', wi_s.ap()[t], wis[:], reads=["awis"], writes=["wi_s"], slot="swi")
                    p4, p5 = bank_bf(4), bank_bf(5)
                    for c in range(8):
                        op('pe', lambda c=c: T_.transpose(out=p4[:, c * 128:(c + 1) * 128],
                                                          in_=qkb[:, c * 128:(c + 1) * 128], identity=identb[:]),
                           reads=["aqkb", "identb"], writes=[BK[4]])
                    for c in range(5):
                        op('pe', lambda c=c: T_.transpose(out=p5[:, c * 128:(c + 1) * 128],
                                                          in_=qkb[:, 1536 + c * 128:1536 + (c + 1) * 128],
                                                          identity=identb[:]),
                           reads=["aqkb", "identb"], writes=[BK[5]])
                    for par in range(2):
                        r0_, r1_ = par * 64, par * 64 + 64
                        src = AP(p4.tensor, p4.offset + r0_ * p4.ap[0][0], [[p4.ap[0][0], 64], [128, 4], [1, 128]])
                        dst = AP(qTz, r0_ * 1024 + par * 128, [[1024, 64], [256, 4], [1, 128]])
                        op('act', lambda src=src, dst=dst: A_.copy(out=dst, in_=src), reads=[BK[4]], writes=["aqTz"])
                        src = AP(p5.tensor, p5.offset + r0_ * p5.ap[0][0], [[p5.ap[0][0], 64], [128, 4], [1, 128]])
                        dst = AP(qiTz, r0_ * 1024 + par * 128, [[1024, 64], [256, 4], [1, 128]])
                        op('dve', lambda src=src, dst=dst: V.tensor_copy(out=dst, in_=src), reads=[BK[5]],
                           writes=["aqiTz"])
                    op('act', lambda: A_.copy(out=kT[:].rearrange("p a b -> p (a b)"), in_=p4[:, 512:1024]),
                       reads=[BK[4]], writes=["akT"])
                    op('dve', lambda: V.tensor_copy(out=kiT[:], in_=p5[:, 512:640]), reads=[BK[5]], writes=["akiT"])
                    dma('pool', qT_s.ap()[t], qTz[:].rearrange("p a b c -> p (a b c)"), reads=["aqTz"],
                        writes=["qT_s"], slot="sq")
                    dma('pool', qiT_s.ap()[t], qiTz[:].rearrange("p a b c -> p (a b c)"), reads=["aqiTz"],
                        writes=["qiT_s"], slot="sqi")
                    dma('pool', kT_s.ap()[b_, :, :, i * 128:(i + 1) * 128], kT[:], reads=["akT"],
                        writes=["kT_s"], slot="sk")
                    dma('pool', kiT_s.ap()[b_, :, i * 128:(i + 1) * 128], kiT[:], reads=["akiT"],
                        writes=["kiT_s"], slot="ski")
                    for g in range(4):
                        op('pe', lambda g=g, pc=pc, i=i: T_.matmul(
                            banks[6][:, g * 128:(g + 1) * 128], lhsT=pc[:, g * 128:(g + 1) * 128],
                            rhs=bandt[:, 0 if i == 0 else 1, g, :], start=True, stop=(i == 0)),
                           reads=[pck, "bandt"], writes=[BK[6]])
                        if i > 0:
                            pv = pp[(i + 1) % 2]
                            op('pe', lambda g=g, pv=pv: T_.matmul(
                                banks[6][:, g * 128:(g + 1) * 128], lhsT=pv[:, g * 128:(g + 1) * 128],
                                rhs=bandt[:, 2, g, :], start=False, stop=True),
                               reads=[f"app{(i + 1) % 2}", "bandt"], writes=[BK[6]])
                    op('act', lambda: A_.copy(out=pooledT[:].rearrange("p a b -> p (a b)"), in_=banks[6][:]),
                       reads=[BK[6]], writes=["apooledT"])
                    for g in range(4):
                        op('pe', lambda g=g: T_.matmul(banks[6][:, g * 128:(g + 1) * 128], lhsT=poolwb[:, g, :],
                                                       rhs=pooledT[:, g, :], start=True, stop=True),
                           reads=["poolwb", "apooledT"], writes=[BK[6]])
                    psb = AP(pscale, 0, [[4, 128], [1, 4], [0, 128]])
                    op('dve', lambda: V.tensor_tensor(out=mixedT[:], in0=banks[6][:].rearrange("p (a b) -> p a b", a=4),
                                                      in1=psb, op=ALU.mult),
                       reads=[BK[6], "pscale"], writes=["amixedT"])
                    for hf in range(2):
                        bi = hf
                        for g in range(4):
                            op('pe', lambda g=g, hf=hf, bi=bi: T_.matmul(
                                banks[bi][:], lhsT=mixedT[:, g, :], rhs=wbpb[:, g, hf * 512:(hf + 1) * 512],
                                start=(g == 0), stop=(g == 3)),
                               reads=["amixedT", "wbpb"], writes=[BK[bi]])
                        op('dve', lambda hf=hf, bi=bi: V.tensor_tensor(
                            out=zpb[:, hf * 512:(hf + 1) * 512], in0=banks[bi][:],
                            in1=gpb[:, hf * 512:(hf + 1) * 512], op=ALU.mult),
                           reads=[BK[bi], "agpb"], writes=["azpb"])
                    dma('pool', zp_s.ap()[t], zpb[:], reads=["azpb"], writes=["zp_s"], slot="szp")
                    dma('pool', ga_s.ap()[t], gab[:], reads=["agab"], writes=["ga_s"], slot="sga")
                pq.run(10 ** 9)
                S_.barrier()

        def mixer_b():
            NIT = 17
            TOPK = min(256, S // 4)
            with ExitStack() as st:
                KT = sb(st, "bKT", [128, 4, S], BF16)
                Va = sb(st, "bVa", [128, TPS, 520], BF16)
                kiT = sb(st, "bkiT", [128, S], BF16)
                wbab = sb(st, "bwbab", [128, 4, D], BF16)
                woutb = sb(st, "bwoutb", [128, 8, D], BF16)
                bmask = sb(st, "bbmask", [128, 128], F32)
                thrc = sb(st, "bthrc", [128, 1], F32)
                cpow = sb(st, "bcpow", [128, NIT + 1], F32)
                score = [sb(st, f"bscore{i}", [128, S], F32) for i in range(2)]
                mneg = [sb(st, f"bmneg{i}", [128, S], BF16) for i in range(2)]
                cj = sb(st, "bcj", [128, S], BF16)
                ident4 = sb(st, "bident4", [128, 4, 128], BF16)
                with ExitStack() as st2:
                    wst = sb(st2, "bwst", [128, 8, D], F32)
                    dma('sp', wst[:], wout_d.ap().rearrange("(a p) c -> p a c", p=128), writes=["bwst"], slot="c5")
                    op('dve', lambda: V.tensor_copy(out=woutb[:], in_=wst[:]), reads=["bwst"], writes=["bwoutb"])
                    dma('sp', wst[:, 0:4, :], wba_d.ap().rearrange("(a p) c -> p a c", p=128), reads=[],
                        writes=["bwst"], slot="c5")
                    op('dve', lambda: V.tensor_copy(out=wbab[:], in_=wst[:, 0:4, :]), reads=["bwst"], writes=["bwbab"])
                    dma('sp', bmask[:], bmask_d.ap(), writes=["bbmask"], slot="c6")
                    op('dve', lambda: V.memset(thrc[:], -1.0e29), writes=["bthrc"])
                    for k in range(4):
                        op('dve', lambda k=k: V.tensor_copy(out=ident4[:, k, :], in_=identb[:]), reads=["identb"], writes=["bident4"])
                    for k in range(NIT + 1):
                        op('dve', lambda k=k: V.memset(cpow[:, k:k + 1], 0.5 ** k), writes=["bcpow"])
                    S_.barrier()
                qTz = [sb(st, f"bqTz{i}", [128, 4, 2, 128], BF16) for i in range(2)]
                qiTz = [sb(st, f"bqiTz{i}", [128, 4, 2, 128], BF16) for i in range(2)]
                wis = [sb(st, f"bwis{i}", [128, 8], F32) for i in range(2)]
                gab = [sb(st, f"bgab{i}", [128, D], BF16) for i in range(2)]
                zpb = [sb(st, f"bzpb{i}", [128, D], BF16) for i in range(2)]
                xt = [sb(st, f"bxt{i}", [128, D], F32) for i in range(2)]
                dg = [sb(st, f"bdg{i}", [128, 8, 128], BF16) for i in range(2)]
                rl = [sb(st, f"brl{i}", [128, 512], BF16) for i in range(2)]
                PT = [sb(st, f"bPT{i}", [128, 512], BF16) for i in range(3)]
                lo = sb(st, "blo", [128, 1], F32)
                w0 = sb(st, "bw0", [128, 1], F32)
                hw2 = sb(st, "bhw2", [128, NIT + 1], F32)
                mid = [sb(st, f"bmid{i}", [128, 1], F32) for i in range(2)]
                cnt = sb(st, "bcnt", [128, 1], F32)
                stp = sb(st, "bstp", [128, 1], F32)
                rsum = sb(st, "brsum", [128, 8], F32)
                attn = sb(st, "battn", [128, 512], BF16)
                attnT = sb(st, "battnT", [128, 4, 128], BF16)
                zf = sb(st, "bzf", [128, D], BF16)
                zb = sb(st, "bzb", [128, D], BF16)
                zT = sb(st, "bzT", [128, 8, 128], BF16)
                HB = {("0", 0): (0, 0), ("0", 1): (0, 256), ("1", 0): (1, 0), ("1", 1): (1, 256)}

                def load_seq_ki(b_):
                    dma('sp', kiT[:], kiT_s.ap()[b_], reads=["kiT_s"], writes=["bkiT"], slot="lki")

                def load_seq_kv(b_):
                    dma('sp', KT[:], kT_s.ap()[b_], reads=["kT_s"], writes=["bKT"], slot="lk")
                    for q in range(0, TPS, 8):
                        q1 = min(TPS, q + 8)
                        dma('sp', Va[:, q:q1, :],
                            v2_s.ap()[b_, q * 128:q1 * 128, :].rearrange("(a p) c -> p a c", p=128),
                            reads=["v2_s"], writes=["bVa"], slot="lv")

                def s1_load_diag(t):
                    b_, i = divmod(t, TPS)
                    L = 128 * (i + 1)
                    d = t % 2
                    sc, sck = score[d], f"bscore{d}"
                    dma('sp', qiTz[d][:].rearrange("p a b c -> p (a b c)"), qiT_s.ap()[t], reads=["qiT_s"],
                        writes=[f"bqiTz{d}"], slot=f"lqi{d}")
                    dma('sp', wis[d][:], wi_s.ap()[t], reads=["wi_s"], writes=[f"bwis{d}"], slot=f"lwi{d}")
                    for h in range(8):
                        op('dve', lambda h=h, d=d: V.tensor_scalar(out=dg[d][:, h, :], in0=identb[:],
                                                                   scalar1=wis[d][:, h:h + 1], scalar2=None,
                                                                   op0=ALU.mult),
                           reads=["identb", f"bwis{d}"], writes=[f"bdg{d}"])

                def s1_index(t):
                    b_, i = divmod(t, TPS)
                    L = 128 * (i + 1)
                    d = t % 2
                    sc, sck = score[d], f"bscore{d}"
                    nch = (L + 511) // 512
                    LB = (0, 5)
                    for c in range(nch):
                        n = min(512, L - 512 * c)

                        def lg(h, c=c, n=n):
                            bi = LB[h % 2]
                            op('pe', lambda: T_.matmul(
                                banks[bi][:, 0:n], lhsT=qiTz[d][:, h // 2, h % 2, :],
                                rhs=kiT[:, 512 * c:512 * c + n], start=True, stop=True),
                               reads=[f"bqiTz{d}", "bkiT"], writes=[BK[bi]])
                            op('act', lambda: A_.activation(out=rl[h % 2][:, 0:n], in_=banks[bi][:, 0:n],
                                                            func=AF.Relu),
                               reads=[BK[bi]], writes=[f"brl{h % 2}"])

                        def ac(h, n=n):
                            op('pe', lambda: T_.matmul(
                                banks[1][:, 0:n], lhsT=dg[d][:, h, :], rhs=rl[h % 2][:, 0:n],
                                start=(h == 0), stop=(h == 7)),
                               reads=[f"bdg{d}", f"brl{h % 2}"], writes=[BK[1]])

                        lg(0)
                        for h in range(8):
                            if h + 1 < 8:
                                lg(h + 1)
                            ac(h)
                        op('act', lambda c=c, n=n, sc=sc: A_.copy(out=sc[:, 512 * c:512 * c + n],
                                                                 in_=banks[1][:, 0:n]),
                           reads=[BK[1]], writes=[sck])

                def s1_bisect(t):
                    b_, i = divmod(t, TPS)
                    L = 128 * (i + 1)
                    d = t % 2
                    sc, sck = score[d], f"bscore{d}"
                    op('dve', lambda L=L, sc=sc: V.tensor_tensor(out=sc[:, L - 128:L], in0=sc[:, L - 128:L],
                                                                 in1=bmask[:], op=ALU.add),
                       reads=[sck, "bbmask"], writes=[sck])
                    if 128 * i + 64 > TOPK:
                        op('dve', lambda L=L, sc=sc: V.tensor_reduce(out=lo[:], in_=sc[:, 0:TOPK], axis=AX.X,
                                                                     op=ALU.min),
                           reads=[sck], writes=["blo"])
                        op('dve', lambda L=L, sc=sc: V.tensor_reduce(out=w0[:], in_=sc[:, 0:L], axis=AX.X, op=ALU.max),
                           reads=[sck], writes=["bw0"])
                        op('dve', lambda: V.tensor_tensor(out=w0[:], in0=w0[:], in1=lo[:], op=ALU.subtract),
                           reads=["bw0", "blo"], writes=["bw0"])
                        w0b = AP(w0, 0, [[1, 128], [0, NIT + 1]])
                        op('dve', lambda w0b=w0b: V.tensor_tensor(out=hw2[:], in0=cpow[:], in1=w0b, op=ALU.mult),
                           reads=["bw0", "bcpow"], writes=["bhw2"])
                        op('dve', lambda: V.scalar_tensor_tensor(out=mid[0][:], in0=w0[:], scalar=0.5, in1=lo[:],
                                                                 op0=ALU.mult, op1=ALU.add),
                           reads=["bw0", "blo"], writes=["bmid0"])
                        nit = min(NIT, max(12, int(np.ceil(np.log2(20.0 * L)))))
                        for it in range(nit):
                            mi, mo = it % 2, (it + 1) % 2
                            op('dve', lambda L=L, sc=sc, mi=mi: V.tensor_scalar(
                                out=cj[:, 0:L], in0=sc[:, 0:L], scalar1=mid[mi][:, 0:1], scalar2=None,
                                op0=ALU.is_ge, op1=ALU.add, accum_out=cnt[:]),
                               reads=[sck, f"bmid{mi}"], writes=["bcj", "bcnt"])
                            last = (it == nit - 1)
                            op('dve', lambda last=last: V.tensor_scalar(
                                out=stp[:], in0=cnt[:], scalar1=TOPK - 0.5, scalar2=(1.0 if last else 0.5),
                                op0=ALU.is_ge, op1=ALU.subtract),
                               reads=["bcnt"], writes=["bstp"])
                            k = nit if last else it + 1
                            op('dve', lambda k=k, mi=mi, mo=mo: V.scalar_tensor_tensor(
                                out=mid[mo][:], in0=stp[:], scalar=hw2[:, k:k + 1], in1=mid[mi][:],
                                op0=ALU.mult, op1=ALU.add),
                               reads=["bstp", "bhw2", f"bmid{mi}"], writes=[f"bmid{mo}"])
                        thr, thrk = mid[nit % 2], f"bmid{nit % 2}"
                    else:
                        thr, thrk = thrc, "bthrc"
                    op('dve', lambda L=L, thr=thr, sc=sc, d=d: V.tensor_scalar(
                        out=mneg[d][:, 0:L], in0=sc[:, 0:L], scalar1=thr[:, 0:1], scalar2=-30000.0,
                        op0=ALU.is_lt, op1=ALU.mult),
                       reads=[sck, thrk], writes=[f"bmneg{d}"])


                def s2_load(t):
                    b_, i = divmod(t, TPS)
                    L = 128 * (i + 1)
                    d = t % 2
                    sc, sck = score[d], f"bscore{d}"
                    dma('sp', qTz[d][:].rearrange("p a b c -> p (a b c)"), qT_s.ap()[t], reads=["qT_s"],
                        writes=[f"bqTz{d}"], slot=f"lq{d}")
                    dma('sp', gab[d][:], ga_s.ap()[t], reads=["ga_s"], writes=[f"bgab{d}"], slot=f"lga{d}")
                    dma('sp', zpb[d][:], zp_s.ap()[t], reads=["zp_s"], writes=[f"bzpb{d}"], slot=f"lzp{d}")
                    dma('sp', xt[d][:], x_d.ap()[t * 128:(t + 1) * 128, :], writes=[f"bxt{d}"], slot=f"lx{d}")

                def stage2(t):
                    b_, i = divmod(t, TPS)
                    d = t % 2
                    SB3 = (2, 3, 4)
                    units = [(kc, half) for kc in range(i + 1) for half in range(2)]

                    def emitS(u):
                        kc, half = units[u]
                        bi = SB3[u % 3]
                        op('pe', lambda: T_.matmul(
                            banks[bi][:], lhsT=mneg[d][:, kc * 128:(kc + 1) * 128],
                            rhs=ident4[:].rearrange("p a b -> p (a b)"), start=True, stop=False),
                           reads=[f"bmneg{d}", "bident4"], writes=[BK[bi]])
                        for hp2 in range(2):
                            hp = half * 2 + hp2
                            op('pe', lambda hp=hp, hp2=hp2: T_.matmul(
                                banks[bi][:, hp2 * 256:(hp2 + 1) * 256], lhsT=KT[:, hp, kc * 128:(kc + 1) * 128],
                                rhs=qTz[d][:, hp, :, :].rearrange("p a b -> p (a b)"), start=False, stop=(hp2 == 1)),
                               reads=["bKT", f"bqTz{d}"], writes=[BK[bi]])

                    def emitP(u):
                        kc, half = units[u]
                        bi = SB3[u % 3]
                        pt = PT[u % 3]
                        ptk = f"bPT{u % 3}"
                        op('act', lambda: A_.activation(out=pt[:], in_=banks[bi][:], func=AF.Exp, scale=0.125),
                           reads=[BK[bi]], writes=[ptk])
                        for j in range(4):
                            h = half * 4 + j
                            bo = 6 + half
                            op('pe', lambda h=h, j=j, bo=bo: T_.matmul(
                                banks[bo][:, j * 65:j * 65 + 65], lhsT=pt[:, j * 128:(j + 1) * 128],
                                rhs=Va[:, kc, h * 65:(h + 1) * 65], start=(kc == 0 and j == 0),
                                stop=(kc == i and j == 3), skip_group_check=True),
                               reads=[ptk, "bVa"], writes=[BK[bo]])

                    emitS(0)
                    for u in range(len(units)):
                        if u + 1 < len(units):
                            emitS(u + 1)
                        emitP(u)
                    for hf in range(2):
                        ov = AP(banks[6 + hf], 0, [[512, 128], [65, 4], [1, 64]])
                        sv = AP(banks[6 + hf], 64, [[512, 128], [65, 4], [1, 1]])
                        op('dve', lambda hf=hf, sv=sv: V.reciprocal(
                            out=rsum[:, hf * 4:(hf + 1) * 4].rearrange("p (a b) -> p a b", b=1), in_=sv),
                           reads=[BK[6 + hf]], writes=["brsum"])
                        rb = AP(rsum, hf * 4, [[8, 128], [1, 4], [0, 64]])
                        av = AP(attn, hf * 256, [[512, 128], [64, 4], [1, 64]])
                        op('dve', lambda ov=ov, rb=rb, av=av: V.tensor_tensor(out=av, in0=ov, in1=rb, op=ALU.mult),
                           reads=[BK[6 + hf], "brsum"], writes=["battn"])
                    p0 = bank_bf(6)
                    for c in range(4):
                        op('pe', lambda c=c: T_.transpose(out=p0[:, c * 128:(c + 1) * 128],
                                                          in_=attn[:, c * 128:(c + 1) * 128], identity=identb[:]),
                           reads=["battn", "identb"], writes=[BK[6]])
                    op('act', lambda: A_.copy(out=attnT[:].rearrange("p a b -> p (a b)"), in_=p0[:, 0:512]),
                       reads=[BK[6]], writes=["battnT"])
                    for hf in range(2):
                        bi = 2 + hf
                        for c in range(4):
                            op('pe', lambda c=c, hf=hf, bi=bi: T_.matmul(
                                banks[bi][:], lhsT=attnT[:, c, :], rhs=wbab[:, c, hf * 512:(hf + 1) * 512],
                                start=(c == 0), stop=(c == 3)),
                               reads=["battnT", "bwbab"], writes=[BK[bi]])
                        op('dve', lambda hf=hf, bi=bi, d=d: V.tensor_tensor(
                            out=zf[:, hf * 512:(hf + 1) * 512], in0=banks[bi][:],
                            in1=gab[d][:, hf * 512:(hf + 1) * 512], op=ALU.mult),
                           reads=[BK[bi], f"bgab{d}"], writes=["bzf"])
                    op('dve', lambda d=d: V.tensor_tensor(out=zb[:], in0=zf[:], in1=zpb[d][:], op=ALU.add),
                       reads=["bzf", f"bzpb{d}"], writes=["bzb"])
                    p1 = bank_bf(7)
                    for c in range(8):
                        op('pe', lambda c=c: T_.transpose(out=p1[:, c * 128:(c + 1) * 128],
                                                          in_=zb[:, c * 128:(c + 1) * 128], identity=identb[:]),
                           reads=["bzb", "identb"], writes=[BK[7]])
                    op('act', lambda: A_.copy(out=zT[:].rearrange("p a b -> p (a b)"), in_=p1),
                       reads=[BK[7]], writes=["bzT"])
                    for hf in range(2):
                        bi = (4, 2)[hf]
                        for c in range(8):
                            op('pe', lambda c=c, hf=hf, bi=bi: T_.matmul(
                                banks[bi][:], lhsT=zT[:, c, :], rhs=woutb[:, c, hf * 512:(hf + 1) * 512],
                                start=(c == 0), stop=(c == 7)),
                               reads=["bzT", "bwoutb"], writes=[BK[bi]])
                        op('dve', lambda hf=hf, bi=bi, d=d: V.tensor_tensor(
                            out=xt[d][:, hf * 512:(hf + 1) * 512], in0=banks[bi][:],
                            in1=xt[d][:, hf * 512:(hf + 1) * 512], op=ALU.add),
                           reads=[BK[bi], f"bxt{d}"], writes=[f"bxt{d}"])
                    dst = h_s if "peer" in phases else y_d
                    dma('pool', dst.ap()[t * 128:(t + 1) * 128, :], xt[d][:], reads=[f"bxt{d}"],
                        writes=["h_s"], slot=f"sh{d}")

                load_seq_ki(0)
                load_seq_kv(0)
                s1_load_diag(0)
                s1_index(0)
                s1_bisect(0)
                s2_load(0)
                if NTILE > 1:
                    if 1 % TPS == 0:
                        load_seq_ki(1 // TPS)
                    s1_load_diag(1)
                    s1_index(1)
                for t in range(NTILE):
                    b_, i = divmod(t, TPS)
                    if t + 2 < NTILE:
                        s1_load_diag(t + 2)
                    if t + 1 < NTILE:
                        s1_bisect(t + 1)
                    if t + 2 < NTILE:
                        if (t + 2) % TPS == 0:
                            load_seq_ki((t + 2) // TPS)
                        s1_index(t + 2)
                    stage2(t)
                    if t + 1 < NTILE:
                        if (t + 1) % TPS == 0:
                            load_seq_kv(b_ + 1)
                        s2_load(t + 1)
                S_.barrier()

        if "mix" in phases:
            mixer_a()
            mixer_b()
        else:
            with ExitStack() as st:
                tb = [sb(st, f"cp{i}", [128, D], F32) for i in range(2)]
                for t in range(NTILE):
                    b = t % 2
                    dma('sp', tb[b][:], x_d.ap()[t * 128:(t + 1) * 128, :], writes=[f"cp{b}"],
                        slot=f"cpi{b}")
                    dma('sp', h_s.ap()[t * 128:(t + 1) * 128, :], tb[b][:], reads=[f"cp{b}"],
                        writes=["h_s"], slot=f"cpo{b}")
                S_.barrier()
        if "peer" in phases:
            peer_phase()
        elif "copy" in phases:
            with ExitStack() as st:
                tb = [sb(st, f"cq{i}", [128, D], F32) for i in range(2)]
                for t in range(NTILE):
                    b = t % 2
                    dma('sp', tb[b][:], h_s.ap()[t * 128:(t + 1) * 128, :], reads=["h_s"], writes=[f"cq{b}"],
                        slot=f"cqi{b}")
                    dma('pool', y_d.ap()[t * 128:(t + 1) * 128, :], tb[b][:], reads=[f"cq{b}"],
                        writes=["y"], slot=f"cqo{b}")
                S_.barrier()
        S_.barrier()
    return nc


def host_consts(S):
    bf = ml_dtypes.bfloat16
    identb = np.eye(128, dtype=np.float32).astype(bf)
    iota128 = np.broadcast_to(np.arange(128, dtype=np.float32)[None, :], (128, 128)).astype(bf)
    c16 = np.zeros((128, 3, 16), np.float32)
    c16[:, 0, :] = 16.0 * np.arange(16)
    c16[:, 1, :] = np.arange(16)
    return dict(identb=identb, iota128=iota128, c16=c16)


def make_in_maps(inputs, n_cores, NB, S):
    f = np.float32
    x = np.asarray(inputs["x"], f)
    B = x.shape[0]
    assert B == n_cores * NB and x.shape[1] == S
    c = host_consts(S)

    def pk(v):
        return np.ascontiguousarray(np.asarray(v, f).reshape(8, 128).T)

    sk = np.asarray(inputs["peer_subkeys"], f)[0]
    skt = np.zeros((2, 64, 8, 2, 128), f)
    for p in range(2):
        skt[p, :, :, p, :] = sk[:, p].transpose(2, 0, 1)
    skt = np.ascontiguousarray(skt.reshape(128, 16, 128))
    half = 8
    inv_freq = (500000.0 ** (-np.arange(half, dtype=np.float32) / half)).astype(f)
    ang = np.arange(S, dtype=f)[:, None] * inv_freq[None, :]
    rope = np.stack([np.cos(ang), np.sin(ang)], axis=1).astype(f).reshape(S // 128, 128, 2, 8)
    gqk = np.stack([np.asarray(inputs["q_norm_g"], f)[0], np.asarray(inputs["k_norm_g"], f)[0]], 0)
    gqk = np.ascontiguousarray(np.broadcast_to(gqk[None], (128, 2, 64)))
    poolw = np.ascontiguousarray(np.asarray(inputs["pool_w"], f)[0].transpose(1, 0, 2))
    pscale = np.ascontiguousarray(np.asarray(inputs["pool_scale"], f)[0].reshape(4, 128).T)
    band = np.zeros((3, 128, 4, 128), f)
    for g, w in enumerate((2, 4, 8, 16)):
        for t in range(128):
            cnt0 = min(t + 1, w)
            for s_ in range(max(0, t - w + 1), t + 1):
                band[0, s_, g, t] += 1.0 / cnt0
                band[1, s_, g, t] += 1.0 / w
            band[0, t, g, t] -= 1.0
            band[1, t, g, t] -= 1.0
            for s_ in range(t - w + 1, 0):
                band[2, 128 + s_, g, t] += 1.0 / w
    bmask = np.zeros((128, 128), f)
    bmask[:64, 64:] = NEG
    shared = dict(
        g2=pk(inputs["norm2_g"][0]), wq=np.ascontiguousarray(np.asarray(inputs["peer_wq"], f)[0]),
        skt=skt, pu=np.ascontiguousarray(np.asarray(inputs["peer_u"], f)[0]),
        pv=np.ascontiguousarray(np.asarray(inputs["peer_v"], f)[0]),
        g1=pk(inputs["norm1_g"][0]), win=np.ascontiguousarray(np.asarray(inputs["w_in"], f)[0]),
        gqk=gqk, rope=rope, poolw=poolw, pscale=pscale,
        wba=np.ascontiguousarray(np.asarray(inputs["w_branch_attn"], f)[0]),
        wbp=np.ascontiguousarray(np.asarray(inputs["w_branch_pool"], f)[0]),
        wout=np.ascontiguousarray(np.asarray(inputs["w_out"], f)[0]),
        band=band, bmask=bmask, **c)
    maps = []
    for i in range(n_cores):
        m = dict(shared)
        m["x"] = np.ascontiguousarray(x[i * NB:(i + 1) * NB].reshape(NB * S, D))
        maps.append(m)
    return maps


_NC_CACHE = {}


def kernel(**inputs):
    n_cores = 8
    x = np.asarray(inputs["x"])
    B, S, _ = x.shape
    NB = B // n_cores
    key = (NB, S)
    if key not in _NC_CACHE:
        _NC_CACHE[key] = build(NB, S)
    nc = _NC_CACHE[key]
    maps = make_in_maps(inputs, n_cores, NB, S)
    res = run_bass_kernel_spmd(nc, maps, core_ids=list(range(n_cores)))
    out = np.stack([r["y"].reshape(NB, S, D) for r in res.results], 0).reshape(B, S, D)
    return out.astype(np.float32)
```
